# Optimizing a Trainium2 kernel written in Bass

```python
import jax, jax.numpy as jnp
from jax import lax
import numpy as np

D_MODEL = 1024
BATCH = 8
SEQ = 4096
DEPTH = 4

POOL_WINDOWS = (2, 4, 8, 16)
POOL_WIDTH = D_MODEL // 4
POOL_GROUP = POOL_WIDTH // len(POOL_WINDOWS)
HGRN_HEADS = 4
HGRN_KDIM = D_MODEL // 16
HGRN_VDIM = D_MODEL // 16
HGRN_WIDTH = HGRN_HEADS * HGRN_VDIM
HGRN_CHUNK = 64
ATT_HEADS = 8
ATT_KV_HEADS = 2
HEAD_DIM = 64
ATT_WIDTH = ATT_HEADS * HEAD_DIM
WINDOW = 128
MIX_WIDTH = POOL_WIDTH + HGRN_WIDTH + ATT_WIDTH
SPLIT_SIZES = (POOL_WIDTH,
               HGRN_HEADS * HGRN_KDIM, HGRN_HEADS * HGRN_KDIM,
               HGRN_HEADS * HGRN_VDIM, HGRN_HEADS * HGRN_VDIM,
               ATT_HEADS * HEAD_DIM, ATT_KV_HEADS * HEAD_DIM, ATT_KV_HEADS * HEAD_DIM)
IN_WIDTH = sum(SPLIT_SIZES)
N_EXPERTS = 16
N_GROUPS = 4
EXPERTS_PER_GROUP = N_EXPERTS // N_GROUPS
TOP_K = 2
D_EXPERT = D_MODEL // 2
EPS = 1e-6
MAX_ONE_MINUS_F = 1.0 - 1e-6

kernel_name = "hybrid_pool_hgrn2_swa_groupmoe_block"


def rms_norm(x, w):
    xf = x.astype(jnp.float32)
    y = xf * lax.rsqrt(jnp.mean(xf * xf, axis=-1, keepdims=True) + EPS)
    return (y * w.astype(jnp.float32)).astype(x.dtype)


def alibi_slopes(n):
    return jnp.exp2(-8.0 * jnp.arange(1, n + 1, dtype=jnp.float32) / n)


def pool_mixer(a, pool_w, pool_scale):
    B, S, _ = a.shape
    af = a.astype(jnp.float32)
    pos = jnp.arange(S, dtype=jnp.float32)[:, None]
    outs = []
    for g, w in enumerate(POOL_WINDOWS):
        ag = af[..., g * POOL_GROUP:(g + 1) * POOL_GROUP]
        cs = jnp.cumsum(ag, axis=1)
        cs0 = jnp.pad(cs, ((0, 0), (1, 0), (0, 0)))
        lower = jnp.pad(cs0[:, :S + 1 - w], ((0, 0), (w - 1, 0), (0, 0)))
        count = jnp.minimum(pos + 1.0, float(w))
        pooled = (cs - lower) / count - ag
        outs.append(jnp.einsum('bsc,cd->bsd', pooled, pool_w[g].astype(jnp.float32)))
    y = jnp.concatenate(outs, axis=-1) * pool_scale.astype(jnp.float32)
    return y.astype(a.dtype)


def hgrn2_mixer(q, f, i, g, lb, norm_w):
    B, S, _ = q.shape
    H, K, V, C = HGRN_HEADS, HGRN_KDIM, HGRN_VDIM, HGRN_CHUNK
    n = S // C
    fz = f.astype(jnp.float32)
    lbf = lb.astype(jnp.float32)
    k = (1.0 - lbf) * jax.nn.sigmoid(-fz)
    log_f = jnp.log1p(-jnp.minimum(k, MAX_ONE_MINUS_F))
    qf = jax.nn.silu(q.astype(jnp.float32))
    vf = i.astype(jnp.float32)

    def to_chunks(t, d):
        return t.reshape(B, n, C, H, d).transpose(1, 0, 3, 2, 4)

    xs = (to_chunks(qf, K), to_chunks(log_f, K), to_chunks(k, K), to_chunks(vf, V))
    causal = (jnp.arange(C)[:, None] >= jnp.arange(C)[None, :])[:, :, None]

    def step(state, inp):
        qc, lfc, kc, vc = inp
        b = jnp.cumsum(lfc, axis=2)
        diff = b[:, :, :, None, :] - b[:, :, None, :, :]
        decay = jnp.exp(jnp.where(causal, diff, -jnp.inf))
        scores = jnp.einsum('bhtk,bhtsk,bhsk->bhts', qc, decay, kc)
        o = (jnp.einsum('bhts,bhsv->bhtv', scores, vc)
             + jnp.einsum('bhtk,bhkv->bhtv', qc * jnp.exp(b), state))
        b_last = b[:, :, -1:, :]
        state = (jnp.exp(b_last[:, :, 0, :])[..., None] * state
                 + jnp.einsum('bhsk,bhsv->bhkv', kc * jnp.exp(b_last - b), vc))
        return state, o

    state0 = jnp.zeros((B, H, K, V), jnp.float32)
    _, o = lax.scan(step, state0, xs)
    o = o.transpose(1, 0, 3, 2, 4).reshape(B, S, H, V)
    o = o * lax.rsqrt(jnp.mean(o * o, axis=-1, keepdims=True) + EPS)
    o = o.reshape(B, S, H * V) * norm_w.astype(jnp.float32) * jax.nn.silu(g.astype(jnp.float32))
    return o.astype(q.dtype)


def swa_attention(q, k, v, q_norm_w, k_norm_w, sinks):
    B, S, _ = q.shape
    Hq, Hkv, hd, W = ATT_HEADS, ATT_KV_HEADS, HEAD_DIM, WINDOW
    G = Hq // Hkv
    nb = S // W
    q = rms_norm(q.reshape(B, S, Hq, hd), q_norm_w)
    k = rms_norm(k.reshape(B, S, Hkv, hd), k_norm_w)
    v = v.reshape(B, S, Hkv, hd)
    qb = q.reshape(B, nb, W, Hkv, G, hd)

    def band(t):
        tb = jnp.pad(t, ((0, 0), (W, 0), (0, 0), (0, 0))).reshape(B, nb + 1, W, Hkv, hd)
        return jnp.concatenate([tb[:, :-1], tb[:, 1:]], axis=2)

    kb, vb = band(k), band(v)
    logits = jnp.einsum('bnqhgd,bnkhd->bnhgqk', qb, kb).astype(jnp.float32) * (hd ** -0.5)
    dist = jnp.arange(W)[:, None] + W - jnp.arange(2 * W)[None, :]
    kpos = jnp.arange(nb)[:, None] * W - W + jnp.arange(2 * W)[None, :]
    valid = ((dist >= 0) & (dist < W))[None] & (kpos >= 0)[:, None, :]
    slopes = alibi_slopes(Hq).reshape(Hkv, G)
    logits = logits - slopes[:, :, None, None] * dist.astype(jnp.float32)
    logits = jnp.where(valid[None, :, None, None], logits, -jnp.inf)
    sink = sinks.astype(jnp.float32).reshape(1, 1, Hkv, G, 1, 1)
    m = jnp.maximum(jnp.max(logits, axis=-1, keepdims=True), sink)
    p = jnp.exp(logits - m)
    p = p / (jnp.sum(p, axis=-1, keepdims=True) + jnp.exp(sink - m))
    o = jnp.einsum('bnhgqk,bnkhd->bnqhgd', p, vb.astype(jnp.float32))
    return o.reshape(B, S, Hq * hd).astype(q.dtype)


def grouped_moe(h, router_w, router_bias, w_gate, w_up, w_down):
    B, S, D = h.shape
    t = h.reshape(B * S, D)
    scores = jax.nn.softmax((t @ router_w).astype(jnp.float32), axis=-1)
    sel = (scores + router_bias.astype(jnp.float32)).reshape(-1, N_GROUPS, EXPERTS_PER_GROUP)
    group_score = jnp.sum(lax.top_k(sel, TOP_K)[0], axis=-1)
    best = jnp.argmax(group_score, axis=-1)
    in_group = (best[:, None] == jnp.arange(N_GROUPS)[None, :])[:, :, None]
    sel = jnp.where(in_group, sel, -jnp.inf).reshape(-1, N_EXPERTS)
    _, idx = lax.top_k(sel, TOP_K)
    w = jnp.take_along_axis(scores, idx, axis=-1)
    w = w / jnp.sum(w, axis=-1, keepdims=True)
    gates = jnp.sum(jax.nn.one_hot(idx, N_EXPERTS, dtype=jnp.float32) * w[..., None], axis=1)
    y = jnp.zeros((B * S, D), jnp.float32)
    for e in range(N_EXPERTS):
        he = jax.nn.silu(t @ w_gate[e]) * (t @ w_up[e])
        y = y + gates[:, e:e + 1] * (he @ w_down[e]).astype(jnp.float32)
    return y.reshape(B, S, D).astype(h.dtype)


def setup_inputs(seed: int = 0) -> dict:
    key = jax.random.key(seed)
    ks = jax.random.split(key, 21)

    def nrm(k, shape, s):
        return jax.random.normal(k, shape, jnp.float32) * s

    return {
        "x": nrm(ks[0], (BATCH, SEQ, D_MODEL), 1.0),
        "c": nrm(ks[1], (BATCH, D_MODEL), 1.0),
        "ada_w": nrm(ks[2], (DEPTH, D_MODEL, 6 * D_MODEL), 0.1 * D_MODEL ** -0.5),
        "ada_b": nrm(ks[3], (DEPTH, 6 * D_MODEL), 0.2),
        "norm1_w": 1.0 + nrm(ks[4], (DEPTH, D_MODEL), 0.05),
        "norm2_w": 1.0 + nrm(ks[5], (DEPTH, D_MODEL), 0.05),
        "w_in": nrm(ks[6], (DEPTH, D_MODEL, IN_WIDTH), D_MODEL ** -0.5),
        "pool_w": nrm(ks[7], (DEPTH, len(POOL_WINDOWS), POOL_GROUP, POOL_GROUP), POOL_GROUP ** -0.5),
        "pool_scale": 1.0 + nrm(ks[8], (DEPTH, POOL_WIDTH), 0.1),
        "hgrn_lb_raw": nrm(ks[9], (DEPTH, HGRN_HEADS * HGRN_KDIM), 0.5),
        "hgrn_norm_w": 1.0 + nrm(ks[10], (DEPTH, HGRN_WIDTH), 0.05),
        "q_norm_w": 1.0 + nrm(ks[11], (DEPTH, HEAD_DIM), 0.05),
        "k_norm_w": 1.0 + nrm(ks[12], (DEPTH, HEAD_DIM), 0.05),
        "attn_sinks": nrm(ks[13], (DEPTH, ATT_HEADS), 1.0),
        "w_out": nrm(ks[14], (DEPTH, MIX_WIDTH, D_MODEL), MIX_WIDTH ** -0.5),
        "router_w": nrm(ks[15], (D_MODEL, N_EXPERTS), D_MODEL ** -0.5),
        "router_bias": nrm(ks[16], (N_EXPERTS,), 0.01),
        "expert_w_gate": nrm(ks[17], (DEPTH, N_EXPERTS, D_MODEL, D_EXPERT), D_MODEL ** -0.5),
        "expert_w_up": nrm(ks[18], (DEPTH, N_EXPERTS, D_MODEL, D_EXPERT), D_MODEL ** -0.5),
        "expert_w_down": nrm(ks[19], (DEPTH, N_EXPERTS, D_EXPERT, D_MODEL), D_EXPERT ** -0.5),
    }


def reference(x, c, ada_w, ada_b, norm1_w, norm2_w, w_in, pool_w, pool_scale, hgrn_lb_raw,
              hgrn_norm_w, q_norm_w, k_norm_w, attn_sinks, w_out, router_w, router_bias,
              expert_w_gate, expert_w_up, expert_w_down):
    p = jax.nn.softmax(hgrn_lb_raw.astype(jnp.float32), axis=0)
    lower_bounds = jnp.maximum(jnp.cumsum(p, axis=0) - p[0:1], 0.0)
    split_points = []
    acc = 0
    for s in SPLIT_SIZES[:-1]:
        acc += s
        split_points.append(acc)
    cond = jax.nn.silu(c)
    for l in range(DEPTH):
        mod = (cond @ ada_w[l] + ada_b[l]).astype(jnp.float32)
        sh1, sc1, g1, sh2, sc2, g2 = jnp.split(mod, 6, axis=-1)
        h = (rms_norm(x, norm1_w[l]).astype(jnp.float32) * (1.0 + sc1[:, None]) + sh1[:, None]).astype(x.dtype)
        z = h @ w_in[l]
        a_in, hq, hf, hi, hg, aq, ak, av = jnp.split(z, split_points, axis=-1)
        y_pool = pool_mixer(a_in, pool_w[l], pool_scale[l])
        y_hgrn = hgrn2_mixer(hq, hf, hi, hg, lower_bounds[l], hgrn_norm_w[l])
        y_attn = swa_attention(aq, ak, av, q_norm_w[l], k_norm_w[l], attn_sinks[l])
        mix = jnp.concatenate([y_pool, y_hgrn, y_attn], axis=-1) @ w_out[l]
        x = x + (g1[:, None] * mix.astype(jnp.float32)).astype(x.dtype)
        h2 = (rms_norm(x, norm2_w[l]).astype(jnp.float32) * (1.0 + sc2[:, None]) + sh2[:, None]).astype(x.dtype)
        y_moe = grouped_moe(h2, router_w, router_bias, expert_w_gate[l], expert_w_up[l], expert_w_down[l])
        x = x + (g2[:, None] * y_moe.astype(jnp.float32)).astype(x.dtype)
    return x
```

```python
import numpy as np
from contextlib import ExitStack

import concourse.bass as bass
import concourse.mybir as mybir
from concourse.bass_utils import run_bass_kernel_spmd

F32 = mybir.dt.float32
BF16 = mybir.dt.bfloat16
AF = mybir.ActivationFunctionType
ALU = mybir.AluOpType
AX = mybir.AxisListType

PE, ACT, DVE, POOL, SP = "pe", "act", "dve", "pool", "sp"
ENGS = (PE, ACT, DVE, POOL, SP)

D = 1024
S = 4096
DEPTH = 4
NE = 16
EPS = 1e-6
MAXK = 1.0 - 1e-6
TA = 256
NTA = S // TA
TB = 2048
NTB = S // TB
SUB = 512
ARENA_WORDS = 52992
NSUB = TB // SUB


class Tracker:
    def __init__(self):
        self.ops = []
        self.lastw = {}
        self.rds = {}
        self.chan_count = {}
        self.last_of_eng = {}
        self.last_of_chan = {}
        self.pending = {}

    def add(self, eng, fn, reads=(), writes=(), chan=None):
        i = len(self.ops)
        deps = {}

        def dep(j, kind):
            if j is None or j == i:
                return
            if deps.get(j) == "RAW":
                return
            deps[j] = kind

        for r in reads:
            dep(self.lastw.get(r), "RAW")
        for w in writes:
            dep(self.lastw.get(w), "WAW")
            for rd in self.rds.get(w, {}).values():
                dep(rd, "WAR")
        for j, k in self.pending.pop(eng, {}).items():
            dep(j, k)
        op = dict(eng=eng, fn=fn, deps=deps, chan=chan)
        if chan is not None:
            n = self.chan_count.get(chan, 0) + 1
            self.chan_count[chan] = n
            op["chan_idx"] = n
            self.last_of_chan[chan] = i
        self.last_of_eng[eng] = i
        self.ops.append(op)
        rkey = eng if chan is None else ("chan", chan)
        for r in reads:
            self.rds.setdefault(r, {})[rkey] = i
        for w in writes:
            self.lastw[w] = i
            self.rds[w] = {}
        return i

    def barrier(self):
        b = {}
        for e, j in self.last_of_eng.items():
            b[j] = "RAW"
        for c, j in self.last_of_chan.items():
            b[j] = "RAW"
        for e in ENGS:
            d = dict(self.pending.get(e, {}))
            d.update(b)
            self.pending[e] = d

    def emit(self, nc, stack):
        ops = self.ops
        need = [False] * len(ops)

        def needs_wait(op, pj, kind):
            if pj["chan"] is not None:
                return True
            if op["chan"] is not None:
                return True
            if pj["eng"] != op["eng"]:
                return True
            if op["eng"] == PE:
                return False
            return True

        for op in ops:
            for j, kind in op["deps"].items():
                if needs_wait(op, ops[j], kind) and ops[j]["chan"] is None:
                    need[j] = True
        cnt = {e: 0 for e in ENGS}
        for i, op in enumerate(ops):
            if op["chan"] is None and need[i]:
                cnt[op["eng"]] += 1
                op["sig"] = cnt[op["eng"]]
        esem = {e: stack.enter_context(nc.semaphore("es_" + e)) for e in ENGS}
        csem = {c: stack.enter_context(nc.semaphore("cs_%d" % k)) for k, c in enumerate(self.chan_count)}
        per_eng = {e: [] for e in ENGS}
        for i, op in enumerate(ops):
            per_eng[op["eng"]].append(i)

        def run(eng_name, eng):
            waited = {}
            for i in per_eng[eng_name]:
                op = ops[i]
                for j, kind in op["deps"].items():
                    pj = ops[j]
                    if not needs_wait(op, pj, kind):
                        continue
                    if pj["chan"] is not None:
                        key, val, sem = ("c", pj["chan"]), 16 * pj["chan_idx"], csem[pj["chan"]]
                    else:
                        key, val, sem = ("e", pj["eng"]), pj["sig"], esem[pj["eng"]]
                    if waited.get(key, 0) >= val:
                        continue
                    waited[key] = val
                    eng.wait_ge(sem, val)
                ins = op["fn"](eng)
                if op["chan"] is not None:
                    ins.then_inc(csem[op["chan"]], 16)
                elif need[i]:
                    ins.then_inc(esem[eng_name], 1)
            if eng_name == SP:
                for c, n in self.chan_count.items():
                    if waited.get(("c", c), 0) < 16 * n:
                        eng.wait_ge(csem[c], 16 * n)
                for e in ENGS:
                    if cnt[e] > 0 and waited.get(("e", e), 0) < cnt[e]:
                        eng.wait_ge(esem[e], cnt[e])

        with nc.Block() as block:
            block.tensor(lambda e: run(PE, e))
            block.scalar(lambda e: run(ACT, e))
            block.vector(lambda e: run(DVE, e))
            block.gpsimd(lambda e: run(POOL, e))
            block.sync(lambda e: run(SP, e))
        return cnt


class Arena:
    def __init__(self, nc, stack, words):
        self.t = stack.enter_context(nc.sbuf_tensor("arena", [128, words], F32))
        self.words = words
        self.off = 0
        self.peak = 0

    def mark(self):
        return self.off

    def reset(self, m):
        self.off = m

    def alloc(self, shape, dt=F32, at=None):
        if at is not None:
            save = self.off
            self.off = at
            ap = self.alloc(shape, dt)
            self.off = save
            return ap
        P = shape[0]
        n = 1
        for s_ in shape[1:]:
            n *= s_
        words = n if dt == F32 else (n + 1) // 2
        words = (words + 1) // 2 * 2
        assert self.off + words <= self.words, ("SBUF arena overflow", self.off, words, self.words)
        ap = self.t[0:P, self.off:self.off + words]
        self.off += words
        self.peak = max(self.peak, self.off)
        if dt != F32:
            ap = ap.bitcast(dt)
        ap = ap[:, 0:n]
        if len(shape) > 2:
            names = ["d%d" % i for i in range(len(shape) - 1)]
            kw = {names[i]: shape[i + 1] for i in range(len(shape) - 1)}
            ap = ap.rearrange("p (%s) -> p %s" % (" ".join(names), " ".join(names)), **kw)
        return ap

def o_mm(out, lhsT, rhs, start=True, stop=True):
    return lambda e: e.matmul(out, lhsT, rhs, start=start, stop=stop)


def o_tr(out, in_, ident):
    return lambda e: e.transpose(out, in_, ident)


def o_act(out, in_, func, bias=None, scale=None):
    kw = {}
    if bias is not None:
        kw["bias"] = bias
    if scale is not None:
        kw["scale"] = scale
    return lambda e: e.activation(out=out, in_=in_, func=func, **kw)


def o_tt(out, in0, in1, op):
    return lambda e: e.tensor_tensor(out=out, in0=in0, in1=in1, op=op)


def o_ts(out, in0, s1, op0, s2=None, op1=None):
    if op1 is None:
        return lambda e: e.tensor_scalar(out=out, in0=in0, scalar1=s1, scalar2=None, op0=op0)
    return lambda e: e.tensor_scalar(out=out, in0=in0, scalar1=s1, scalar2=s2, op0=op0, op1=op1)


def o_stt(out, in0, scalar, in1, op0, op1):
    return lambda e: e.scalar_tensor_tensor(out=out, in0=in0, scalar=scalar, in1=in1, op0=op0, op1=op1)


def o_copy(out, in_):
    return lambda e: e.tensor_copy(out=out, in_=in_)


def o_recip(out, in_):
    return lambda e: e.reciprocal(out=out, in_=in_)


def o_red(out, in_, op):
    return lambda e: e.tensor_reduce(out=out, in_=in_, axis=AX.X, op=op)


def o_memset(ap, v):
    return lambda e: e.memset(ap, v)


def o_dma(out, in_):
    return lambda e: e.dma_start(out=out, in_=in_)


def o_scan(out, d0, d1, init, op0, op1):
    return lambda e: e.tensor_tensor_scan(out=out, data0=d0, data1=d1, initial=init, op0=op0, op1=op1)


def bc(ap, axis, shape):
    return ap.unsqueeze(axis).broadcast_to(list(shape))


def build_program(n_layers=DEPTH, do_b=True, taps=(), stop_a_tiles=None):
    nc = bass.Bass("TRN2", target_bir_lowering=False)
    T = Tracker()
    st = ExitStack()

    def din(name, shape):
        return nc.dram_tensor(name, list(shape), F32, kind="ExternalInput").ap()

    xT_d = din("xT", [D, S])
    c_d = din("c_in", [128, 8])
    adaw_d = din("ada_w", [DEPTH, D, 6 * D])
    adab_d = din("ada_b", [128, DEPTH, 48])
    n1w_d = din("n1w", [128, DEPTH, 8])
    n2w_d = din("n2w", [128, DEPTH, 8])
    win_d = din("w_in", [DEPTH, 128, 8, 2048])
    pwbd_d = din("pwbd", [128, DEPTH, 2, 128])
    pscale_d = din("pscale", [128, DEPTH, 2])
    lbraw_d = din("lbraw", [64, DEPTH, 4])
    hnw_d = din("hnw", [64, DEPTH, 4])
    qnw_d = din("qnw", [128, DEPTH])
    knw_d = din("knw", [128, DEPTH])
    sinks_d = din("sinks", [128, DEPTH, 8])
    wop_d = din("wo_p", [DEPTH, 128, 2, D])
    woh_d = din("wo_h", [DEPTH, 64, 4, D])
    woa_d = din("wo_a", [DEPTH, 128, 4, D])
    rw_d = din("rw", [128, 8, NE])
    rb_d = din("rb", [128, NE])
    wg_d = din("wg", [DEPTH, NE, 128, 8, 512]) if do_b else None
    wu_d = din("wu", [DEPTH, NE, 128, 8, 512]) if do_b else None
    wd_d = din("wd", [DEPTH, NE, 128, 4, D]) if do_b else None
    ident_d = din("ident", [128, 128])
    emask_d = din("emask", [128, 2, 8, 128])
    cmask_d = din("cmask", [64, 64])
    sel_d = din("sel", [48, NE, 128])
    cinv_d = din("cinv", [128, 2, 2, TA])
    bones_d = din("bones", [128, 128])
    invw_d = din("invw", [128, 2])
    y_d = nc.dram_tensor("y", [D, S], F32, kind="ExternalOutput").ap()
    tap_d = {}
    for name, shape in taps:
        tap_d[name] = nc.dram_tensor("tap_" + name, list(shape), F32, kind="ExternalOutput").ap()

    arena = Arena(nc, st, ARENA_WORDS)

    def sb(name, shape, dt=F32):
        return arena.alloc(list(shape), dt)

    ps = [st.enter_context(nc.psum_tensor("ps%d" % b, [128, 512], F32)) for b in range(8)]

    def PSR(b):
        return ("ps", b)

    identf = sb("identf", [128, 128])
    identb = sb("identb", [128, 128], BF16)
    onesb = sb("onesb", [128, 128], BF16)
    bonesb = sb("bonesb", [128, 128], BF16)
    cmask = sb("cmask", [64, 64])
    sel2 = sb("sel2", [48, NE, 128], BF16)
    P6 = sb("P6", [128, DEPTH, 6, 8])
    modp = sb("modp", [128, DEPTH, 48])
    n1w = sb("n1w", [128, DEPTH, 8])
    n2w = sb("n2w", [128, DEPTH, 8])
    pscale = sb("pscale", [128, DEPTH, 2])
    lbm = sb("lbm", [64, DEPTH, 4])
    hnw = sb("hnw", [64, DEPTH, 4])
    qnw = sb("qnw", [128, DEPTH])
    knw = sb("knw", [128, DEPTH])
    esink = sb("esink", [128, DEPTH, 8])
    rw = sb("rw", [128, 8, NE])
    rb = sb("rb", [128, NE])
    cond = sb("cond", [128, 8])

    ndma = [0]

    def load(dst, src, res, eng=SP):
        T.add(eng, o_dma(dst, src), reads=(), writes=(res,), chan="ld_" + str(res))

    def tap(name, src_ap, res, dst=None):
        if name not in tap_d:
            return
        ndma[0] += 1
        T.add(POOL, o_dma(tap_d[name] if dst is None else dst, src_ap), reads=(res,), writes=(("tap", name),),
              chan="tap%d" % ndma[0])

    load(identf[:], ident_d, "identf")
    load(identb[:], ident_d, "identb", eng=POOL)
    load(bonesb[:], bones_d, "bonesb", eng=POOL)
    load(cmask[:], cmask_d, "cmask")
    load(sel2[:], sel_d, "sel2", eng=POOL)
    load(n1w[:], n1w_d, "n1w")
    load(n2w[:], n2w_d, "n2w")
    load(pscale[:], pscale_d, "pscale")
    load(hnw[:], hnw_d, "hnw")
    load(qnw[:], qnw_d, "qnw")
    load(knw[:], knw_d, "knw")
    load(esink[:], sinks_d, "esink")
    load(rw[:], rw_d, "rw")
    load(rb[:], rb_d, "rb")
    load(cond[:], c_d, "cond")
    T.add(DVE, o_memset(onesb[:], 1.0), writes=("onesb",))
    T.add(ACT, o_act(cond[:], cond[:], AF.Silu), reads=("cond",), writes=("cond",))
    T.add(ACT, o_act(esink[:], esink[:], AF.Exp), reads=("esink",), writes=("esink",))

    if True:
        mark0 = arena.mark()

        def sb0(name, shape, dt=F32):
            return arena.alloc(list(shape), dt)

        adab = sb0("adab", [128, DEPTH, 48])
        adaw = [sb0("adaw%d" % i, [128, 8, 512]) for i in range(4)]
        modrow = sb0("modrow", [1, 6 * D])
        lbr = sb0("lbr", [64, DEPTH, 4])
        lbs = sb0("lbs", [64, 4])
        load(adab[:], adab_d, "adab")
        load(lbr[:], lbraw_d, "lbr")
        nblk = 0
        for l in range(n_layers):
            for nb in range(12):
                k = nblk % 4
                nblk += 1
                load(adaw[k][:], adaw_d[l, :, nb * 512:(nb + 1) * 512].rearrange("(c p) n -> p c n", p=128),
                     ("adaw", k), eng=(SP, POOL, SP, POOL)[k])
                for c in range(8):
                    T.add(PE, o_mm(ps[k][0:1, :], cond[:, c:c + 1], adaw[k][:, c, :], start=(c == 0), stop=(c == 7)),
                          reads=(("adaw", k), "cond"), writes=(PSR(k),))
                T.add(DVE, o_copy(modrow[:, nb * 512:(nb + 1) * 512], ps[k][0:1, :]), reads=(PSR(k),),
                      writes=("modrow",))
            for f in range(48):
                T.add(PE, o_tr(ps[4][:, f:f + 1], modrow[0:1, f * 128:(f + 1) * 128], identf[0:1, 0:1]),
                      reads=("modrow", "identf"), writes=(PSR(4),))
            T.add(DVE, o_tt(modp[:, l, :], ps[4][:, 0:48], adab[:, l, :], ALU.add), reads=(PSR(4), "adab"),
                  writes=("modp",))
            T.add(DVE, o_stt(P6[:, l, 0, :], modp[:, l, 8:16], 1.0, n1w[:, l, :], ALU.add, ALU.mult),
                  reads=("modp", "n1w"), writes=("P6",))
            T.add(DVE, o_copy(P6[:, l, 1, :], modp[:, l, 0:8]), reads=("modp",), writes=("P6",))
            T.add(DVE, o_copy(P6[:, l, 2, :], modp[:, l, 16:24]), reads=("modp",), writes=("P6",))
            T.add(DVE, o_stt(P6[:, l, 3, :], modp[:, l, 32:40], 1.0, n2w[:, l, :], ALU.add, ALU.mult),
                  reads=("modp", "n2w"), writes=("P6",))
            T.add(DVE, o_copy(P6[:, l, 4, :], modp[:, l, 24:32]), reads=("modp",), writes=("P6",))
            T.add(DVE, o_copy(P6[:, l, 5, :], modp[:, l, 40:48]), reads=("modp",), writes=("P6",))
        T.add(ACT, o_act(lbr[:], lbr[:], AF.Exp), reads=("lbr",), writes=("lbr",))
        T.add(DVE, o_tt(lbs[:], lbr[:, 0, :], lbr[:, 1, :], ALU.add), reads=("lbr",), writes=("lbs",))
        T.add(DVE, o_tt(lbs[:], lbs[:], lbr[:, 2, :], ALU.add), reads=("lbr", "lbs"), writes=("lbs",))
        T.add(DVE, o_tt(lbs[:], lbs[:], lbr[:, 3, :], ALU.add), reads=("lbr", "lbs"), writes=("lbs",))
        T.add(DVE, o_recip(lbs[:], lbs[:]), reads=("lbs",), writes=("lbs",))
        T.add(DVE, o_tt(lbr[:], lbr[:], bc(lbs[:], 1, [64, DEPTH, 4]), ALU.mult), reads=("lbr", "lbs"),
              writes=("lbr",))
        T.add(DVE, o_memset(lbm[:, 0, :], 0.0), writes=("lbm",))
        T.add(DVE, o_copy(lbm[:, 1, :], lbr[:, 1, :]), reads=("lbr",), writes=("lbm",))
        T.add(DVE, o_tt(lbm[:, 2, :], lbm[:, 1, :], lbr[:, 2, :], ALU.add), reads=("lbr", "lbm"), writes=("lbm",))
        T.add(DVE, o_tt(lbm[:, 3, :], lbm[:, 2, :], lbr[:, 3, :], ALU.add), reads=("lbr", "lbm"), writes=("lbm",))
        T.add(DVE, o_ts(lbm[:], lbm[:], 0.0, ALU.max), reads=("lbm",), writes=("lbm",))
        T.add(DVE, o_ts(lbm[:], lbm[:], -1.0, ALU.mult, 1.0, ALU.add), reads=("lbm",), writes=("lbm",))
        tap("P6", P6[:, 0:n_layers], "P6")
        tap("lbm", lbm[:], "lbm")
        T.barrier()
        arena.reset(mark0)

    for l in range(n_layers):
        src_d = xT_d if l == 0 else y_d
        phase_a(nc, arena, T, ps, PSR, l, src_d, y_d, dict(
            identb=identb, onesb=onesb, bonesb=bonesb, cmask=cmask, P6=P6, pscale=pscale, lbm=lbm, hnw=hnw,
            qnw=qnw, knw=knw, esink=esink, win_d=win_d, pwbd_d=pwbd_d, wop_d=wop_d, woh_d=woh_d, woa_d=woa_d,
            emask_d=emask_d, cinv_d=cinv_d, invw_d=invw_d), tap, load, stop_a_tiles)
        T.barrier()
        if do_b:
            phase_b(nc, arena, T, ps, PSR, l, y_d, dict(
                identb=identb, onesb=onesb, sel2=sel2, P6=P6, rw=rw, rb=rb, wg_d=wg_d, wu_d=wu_d, wd_d=wd_d),
                tap, load)
            T.barrier()

    cnt = T.emit(nc, st)
    st.close()
    return nc, cnt, (len(T.ops), arena.peak)


def interleave(gens):
    gens = [g for g in gens if g is not None]
    while gens:
        for g in list(gens):
            try:
                next(g)
            except StopIteration:
                gens.remove(g)


def drain(g):
    for _ in g:
        pass


def phase_a(nc, arena, T, ps, PSR, l, src_d, y_d, G, tap, load, stop_a_tiles, pipeline=True):
    identb, onesb, bonesb, cmask, P6 = G["identb"], G["onesb"], G["bonesb"], G["cmask"], G["P6"]
    pscale, lbm, hnw, qnw, knw, esink = G["pscale"], G["lbm"], G["hnw"], G["qnw"], G["knw"], G["esink"]
    mark = arena.mark()

    def sb(name, shape, dt=F32):
        return arena.alloc(list(shape), dt)

    NCH = TA // 64
    NBK = TA // 128
    NAV = 6
    win = sb("win", [128, 8, 2048], BF16)
    wop = sb("wop", [128, 2, D], BF16)
    woh = sb("woh", [64, 4, D], BF16)
    woa = sb("woa", [128, 4, D], BF16)
    pwbd = sb("pwbd", [128, 2, 128], BF16)
    emask = sb("emask", [128, 2, 8, 128], BF16)
    cinv = sb("cinv", [128, 2, TA])
    invw = sb("invw", [128, 2])
    xt = [sb("xt%d" % i, [128, 8, TA]) for i in range(2)]
    sq = sb("sq", [128, 8, TA], BF16)
    hT = sb("hT", [128, 8, TA], BF16)
    tmp = sb("tmp", [128, 8, TA])
    rstd = sb("rstd", [128, TA])
    abuf = sb("abuf", [128, 2, 16 + TA])
    s2 = sb("s2", [128, 2, 16 + TA])
    s4 = sb("s4", [128, 2, 16 + TA])
    s8 = sb("s8", [128, 16 + TA])
    s16 = sb("s16", [128, 16 + TA])
    pooled = sb("pooled", [128, 2, TA])
    pooledb = sb("pooledb", [128, 2, TA], BF16)
    ypT = sb("ypT", [128, 2, TA], BF16)
    off_hA = arena.mark()
    hA2 = [sb("hA%d" % i, [64, 4, TA]) for i in range(2)]
    qs2 = [sb("qs%d" % i, [64, 4, TA]) for i in range(2)]
    wohf = arena.alloc([64, 4, D], F32, at=off_hA)
    gs2 = [sb("gs%d" % i, [64, 4, TA]) for i in range(2)]
    hv2 = [sb("hv%d" % i, [64, TA // 64, 256], BF16) for i in range(2)]
    hK = sb("hK", [64, 4, TA])
    hB = sb("hB", [64, 4, TA])
    hC = sb("hC", [64, 4, TA])
    hE = sb("hE", [64, 4, TA])
    onesf = sb("onesf", [64, TA])
    qtl = sb("qtl", [64, 4, TA], BF16)
    ktl = sb("ktl", [64, 4, TA], BF16)
    ktok = sb("ktok", [64, 4, 64], BF16)
    scT = sb("scT", [64, 4, 64], BF16)
    Up = sb("Up", [64, 4, 64])
    Sst = sb("Sst", [64, 4, 64])
    Stl = sb("Stl", [64, 4, 64], BF16)
    eL = sb("eL", [64, 4, NCH])
    eLm = sb("eLm", [64, 4, NCH])
    eM = sb("eM", [64, 4, NCH])
    yhT = sb("yhT", [64, 4, TA], BF16)
    oT = hC
    rso = hE
    sqo = hB.bitcast(BF16)[:, :, 0:TA]
    aq2 = [sb("aq%d" % i, [128, 4, TA]) for i in range(2)]
    sqa2 = [sb("sqa%d" % i, [128, 4, TA], BF16) for i in range(2)]
    ak2 = [sb("ak%d" % i, [128, TA]) for i in range(2)]
    sqk2 = [sb("sqk%d" % i, [128, TA], BF16) for i in range(2)]
    rq = sb("rq", [128, 4, TA])
    aqn = sb("aqn", [128, 4, TA], BF16)
    rk = sb("rk", [128, TA])
    akn = sb("akn", [128, 4, 128], BF16)
    av = sb("av", [128, NAV, 2, 65], BF16)
    pex = [sb("pex%d" % i, [128, 512]) for i in range(2)]
    pT = sb("pT", [128, 2, 8, 128], BF16)
    den = sb("den", [128, 8])
    ytok = sb("ytok", [128, 8, 64], BF16)
    yaT = sb("yaT", [128, 4, TA], BF16)

    for q in range(4):
        load(win[:, :, q * 512:(q + 1) * 512], G["win_d"][l, :, :, q * 512:(q + 1) * 512], ("win", q), eng=POOL)
    HALL = (("hA", 0), ("hA", 1), ("qs", 0), ("qs", 1))
    T.add(SP, o_dma(wohf[:], G["woh_d"][l]), reads=(), writes=("wohf",) + HALL, chan="ld_wohf")
    load(wop[:], G["wop_d"][l], "wop", eng=POOL)
    load(woa[:], G["woa_d"][l], "woa", eng=POOL)
    load(pwbd[:], G["pwbd_d"][:, l], "pwbd", eng=POOL)
    load(emask[:], G["emask_d"], "emask", eng=POOL)
    load(cinv[:], G["cinv_d"][:, 0], "cinv")
    load(invw[:], G["invw_d"], "invw")
    T.add(DVE, o_tt(woh[:], wohf[:], bc(hnw[:, l, :], 2, [64, 4, D]), ALU.mult), reads=("wohf", "hnw"),
          writes=("woh",) + HALL)
    T.add(DVE, o_memset(abuf[:, :, 0:16], 0.0), writes=("abuf",))
    T.add(DVE, o_memset(Sst[:], 0.0), writes=("Sst",))
    T.add(DVE, o_memset(onesf[:], 1.0), writes=("onesf",))
    T.add(DVE, o_memset(av[:, :, :, 64:65], 1.0), writes=("av",))
    WIN = tuple(("win", q) for q in range(4))

    A1 = P6[:, l, 0, :]
    B1 = P6[:, l, 1, :]
    g1 = P6[:, l, 2, :]
    xdv = src_d.rearrange("(c p) t -> p c t", p=128)
    ydv = y_d.rearrange("(c p) t -> p c t", p=128)
    ntiles = NTA if stop_a_tiles is None else stop_a_tiles

    def load_x(j):
        load(xt[j % 2][:], xdv[:, :, j * TA:(j + 1) * TA], ("xt", j % 2))

    def front(j):
        par = j % 2
        xs = xt[par]
        XR = ("xt", par)
        hA, qs, gs, hv = hA2[par], qs2[par], gs2[par], hv2[par]
        aq, sqa, ak, sqk = aq2[par], sqa2[par], ak2[par], sqk2[par]
        T.add(ACT, o_act(sq[:], xs[:], AF.Square), reads=(XR,), writes=("sq",))
        for c in range(8):
            T.add(PE, o_mm(ps[0][:, 0:TA], onesb[:], sq[:, c, :], start=(c == 0), stop=(c == 7)),
                  reads=("sq", "onesb"), writes=(PSR(0),))
        T.add(ACT, o_act(rstd[:], ps[0][:, 0:TA], AF.Ln, bias=EPS, scale=1.0 / D), reads=(PSR(0),), writes=("rstd",))
        T.add(ACT, o_act(rstd[:], rstd[:], AF.Exp, scale=-0.5), reads=("rstd",), writes=("rstd",))
        yield
        T.add(DVE, o_tt(tmp[:], xs[:], bc(rstd[:], 1, [128, 8, TA]), ALU.mult), reads=(XR, "rstd"), writes=("tmp",))
        for c in range(8):
            T.add(POOL, o_ts(hT[:, c, :], tmp[:, c, :], A1[:, c:c + 1], ALU.mult, B1[:, c:c + 1], ALU.add),
                  reads=("tmp", "P6"), writes=("hT",))
        yield
        if j == 0:
            tap("hT", hT[:], "hT")
        rr = [0]

        def bank():
            rr[0] = (rr[0] + 1) % 3
            return rr[0]

        def proj(b, col0, ncol, off, M=128):
            for c in range(8):
                T.add(PE, o_mm(ps[b][0:M, off:off + TA], win[:, c, col0:col0 + ncol], hT[:, c, :],
                               start=(c == 0), stop=(c == 7)), reads=WIN + ("hT",), writes=(PSR(b),))

        def view2(b, M=128):
            return ps[b][0:M, 0:2 * TA].rearrange("p (c t) -> p c t", c=2)

        for hp in range(2):
            b = bank()
            proj(b, 512 + (2 * hp) * 64, 64, 0, M=64)
            proj(b, 512 + (2 * hp + 1) * 64, 64, TA, M=64)
            T.add(ACT, o_act(hA[:, 2 * hp:2 * hp + 2, :], view2(b, 64), AF.Exp), reads=(PSR(b),), writes=(("hA", par),))
            yield
        b = bank()
        proj(b, 0, 128, 0)
        proj(b, 128, 128, TA)
        T.add(ACT, o_act(abuf[:, :, 16:16 + TA], view2(b), AF.Copy), reads=(PSR(b),), writes=("abuf",))
        yield
        for qp in range(2):
            b = bank()
            proj(b, 1280 + (2 * qp) * 128, 128, 0)
            proj(b, 1280 + (2 * qp + 1) * 128, 128, TA)
            T.add(ACT, o_act(aq[:, 2 * qp:2 * qp + 2, :], view2(b), AF.Copy), reads=(PSR(b),), writes=(("aq", par),))
            T.add(ACT, o_act(sqa[:, 2 * qp:2 * qp + 2, :], view2(b), AF.Square), reads=(PSR(b),),
                  writes=(("sqa", par),))
            yield
        b = bank()
        proj(b, 1792, 128, 0)
        T.add(ACT, o_act(ak[:], ps[b][:, 0:TA], AF.Copy), reads=(PSR(b),), writes=(("ak", par),))
        T.add(ACT, o_act(sqk[:], ps[b][:, 0:TA], AF.Square), reads=(PSR(b),), writes=(("sqk", par),))
        yield
        for cp in range(NCH // 2):
            b = bank()
            for k2 in range(2):
                cc = cp * 2 + k2
                for c in range(8):
                    T.add(PE, o_mm(ps[b][0:64, k2 * 256:(k2 + 1) * 256], hT[:, c, cc * 64:(cc + 1) * 64],
                                   win[:, c, 768:1024], start=(c == 0), stop=(c == 7)),
                          reads=WIN + ("hT",), writes=(PSR(b),))
            T.add(DVE, o_copy(hv[:, cp * 2:cp * 2 + 2, :], ps[b][0:64, :].rearrange("p (c t) -> p c t", c=2)),
                  reads=(PSR(b),), writes=(("hv", par),))
            yield
        b = bank()
        for bk in range(NBK):
            for c in range(8):
                T.add(PE, o_mm(ps[b][:, bk * 128:(bk + 1) * 128], hT[:, c, bk * 128:(bk + 1) * 128],
                               win[:, c, 1920:2048], start=(c == 0), stop=(c == 7)),
                      reads=WIN + ("hT",), writes=(PSR(b),))
        slot0 = (j * NBK) % NAV
        T.add(DVE, o_copy(av[:, slot0:slot0 + NBK, :, 0:64],
                          ps[b][:, 0:NBK * 128].rearrange("p (b k d) -> p b k d", b=NBK, k=2)),
              reads=(PSR(b),), writes=("av",))
        yield
        for hp in range(2):
            b = bank()
            proj(b, 256 + (2 * hp) * 64, 64, 0, M=64)
            proj(b, 256 + (2 * hp + 1) * 64, 64, TA, M=64)
            T.add(ACT, o_act(qs[:, 2 * hp:2 * hp + 2, :], view2(b, 64), AF.Silu), reads=(PSR(b),), writes=(("qs", par),))
            yield
        for hp in range(2):
            b = bank()
            proj(b, 1024 + (2 * hp) * 64, 64, 0, M=64)
            proj(b, 1024 + (2 * hp + 1) * 64, 64, TA, M=64)
            T.add(ACT, o_act(gs[:, 2 * hp:2 * hp + 2, :], view2(b, 64), AF.Silu), reads=(PSR(b),), writes=(("gs", par),))
            yield

    def pool_pre(j):
        W = 16 + TA
        T.add(POOL, o_tt(s2[:, :, 1:W], abuf[:, :, 1:W], abuf[:, :, 0:W - 1], ALU.add), reads=("abuf",), writes=("s2",))
        T.add(POOL, o_tt(s4[:, :, 3:W], s2[:, :, 3:W], s2[:, :, 1:W - 2], ALU.add), reads=("s2",), writes=("s4",))
        T.add(POOL, o_tt(s8[:, 7:W], s4[:, 1, 7:W], s4[:, 1, 3:W - 4], ALU.add), reads=("s4",), writes=("s8",))
        T.add(POOL, o_tt(s16[:, 15:W], s8[:, 15:W], s8[:, 7:W - 8], ALU.add), reads=("s8",), writes=("s16",))
        srcs = [(s2[0:64, 0, 16:W], slice(0, 64), 0, "s2"), (s4[64:128, 0, 16:W], slice(64, 128), 0, "s4"),
                (s8[0:64, 16:W], slice(0, 64), 1, "s8"), (s16[64:128, 16:W], slice(64, 128), 1, "s16")]
        for sap, rows, cp, rn in srcs:
            if j == 0:
                T.add(POOL, o_tt(pooled[rows, cp, :], sap, cinv[rows, cp, :], ALU.mult), reads=(rn, "cinv"),
                      writes=("pooled",))
            else:
                T.add(POOL, o_ts(pooled[rows, cp, :], sap, invw[rows, cp:cp + 1], ALU.mult, 0.0, ALU.add),
                      reads=(rn, "invw"), writes=("pooled",))
        T.add(POOL, o_tt(pooledb[:], pooled[:], abuf[:, :, 16:W], ALU.subtract), reads=("pooled", "abuf"),
              writes=("pooledb",))
        T.add(POOL, o_copy(abuf[:, :, 0:16], abuf[:, :, TA:TA + 16]), reads=("abuf",), writes=("abuf",))
        if j == 0:
            tap("pooled", pooledb[:], "pooledb")

    def pool_post(j, b):
        for cp in range(2):
            T.add(PE, o_mm(ps[b][:, cp * TA:(cp + 1) * TA], pwbd[:, cp, :], pooledb[:, cp, :]),
                  reads=("pwbd", "pooledb"), writes=(PSR(b),))
        for cp in range(2):
            T.add(ACT, o_act(ypT[:, cp, :], ps[b][:, cp * TA:(cp + 1) * TA], AF.Copy, scale=pscale[:, l, cp:cp + 1]),
                  reads=(PSR(b), "pscale"), writes=("ypT",))

    def hgrn(j):
        par = j % 2
        hA, qs, gs, hv = hA2[par], qs2[par], gs2[par], hv2[par]
        HA = ("hA", par)
        BA, BB = 3, 4
        T.add(ACT, o_act(hA[:], hA[:], AF.Ln, bias=1.0), reads=(HA,), writes=(HA,))
        T.add(ACT, o_act(hA[:], hA[:], AF.Exp, scale=-1.0), reads=(HA,), writes=(HA,))
        yield
        T.add(DVE, o_tt(hK[:], hA[:], bc(lbm[:, l, :], 2, [64, 4, TA]), ALU.mult), reads=(HA, "lbm"), writes=("hK",))
        T.add(DVE, o_ts(hA[:], hK[:], MAXK, ALU.min), reads=("hK",), writes=(HA,))
        T.add(ACT, o_act(hB[:], hA[:], AF.Ln, bias=1.0, scale=-1.0), reads=(HA,), writes=("hB",))
        yield
        for h in range(4):
            T.add(DVE, o_scan(hC[:, h, :], onesf[:], hB[:, h, :], 0.0, ALU.mult, ALU.add), reads=("hB", "onesf"),
                  writes=("hC",))
        yield
        hC4 = hC[:].rearrange("p h (c t) -> p h c t", t=64)
        hA4 = hA[:].rearrange("p h (c t) -> p h c t", t=64)
        hB4 = hB[:].rearrange("p h (c t) -> p h c t", t=64)
        T.add(DVE, o_copy(hA4[:, :, 0, :], hC4[:, :, 0, :]), reads=("hC",), writes=(HA,))
        T.add(DVE, o_tt(hA4[:, :, 1:NCH, :], hC4[:, :, 1:NCH, :],
                        bc(hC4[:, :, 0:NCH - 1, 63], 3, [64, 4, NCH - 1, 64]), ALU.subtract), reads=("hC",), writes=(HA,))
        yield
        T.add(ACT, o_act(eL[:], hA4[:, :, :, 63], AF.Exp), reads=(HA,), writes=("eL",))
        T.add(ACT, o_act(eM[:], hA4[:, :, :, 31], AF.Exp), reads=(HA,), writes=("eM",))
        T.add(DVE, o_tt(eLm[:], hA4[:, :, :, 63], hA4[:, :, :, 31], ALU.subtract), reads=(HA,), writes=("eLm",))
        T.add(ACT, o_act(eLm[:], eLm[:], AF.Exp), reads=("eLm",), writes=("eLm",))
        T.add(DVE, o_tt(hB4, hA4, bc(hA4[:, :, :, 31], 3, [64, 4, NCH, 64]), ALU.subtract), reads=(HA,), writes=("hB",))
        yield
        T.add(ACT, o_act(hE[:], hB[:], AF.Exp), reads=("hB",), writes=("hE",))
        T.add(ACT, o_act(hC[:], hB[:], AF.Exp, scale=-1.0), reads=("hB",), writes=("hC",))
        yield
        T.add(DVE, o_tt(qtl[:], qs[:], hE[:], ALU.mult), reads=(("qs", par), "hE"), writes=("qtl",))
        T.add(DVE, o_tt(ktl[:], hK[:], hC[:], ALU.mult), reads=("hK", "hC"), writes=("ktl",))
        yield
        for cc in range(NCH):
            cs = slice(cc * 64, (cc + 1) * 64)
            psab = ps[BA][0:64, 0:128].bitcast(BF16)
            for h in range(4):
                T.add(PE, o_tr(psab[:, h * 64:(h + 1) * 64], ktl[:, h, cs], identb[0:64, 0:64]),
                      reads=("ktl", "identb"), writes=(PSR(BA),))
            for h in range(4):
                T.add(PE, o_mm(ps[BB][0:64, h * 64:(h + 1) * 64], ktl[:, h, cs], qtl[:, h, cs]),
                      reads=("ktl", "qtl"), writes=(PSR(BB),))
            T.add(ACT, o_act(ktok[:], psab.rearrange("p (h k) -> p h k", h=4), AF.Copy), reads=(PSR(BA),),
                  writes=("ktok",))
            T.add(DVE, o_tt(scT[:], ps[BB][0:64, 0:256].rearrange("p (h t) -> p h t", h=4),
                            bc(cmask[:], 1, [64, 4, 64]), ALU.mult), reads=(PSR(BB), "cmask"), writes=("scT",))
            T.add(DVE, o_tt(Stl[:], Sst[:], bc(eM[:, :, cc], 2, [64, 4, 64]), ALU.mult), reads=("Sst", "eM"),
                  writes=("Stl",))
            yield
            for h in range(4):
                T.add(PE, o_mm(ps[BA][0:64, h * 64:(h + 1) * 64], ktok[:, h, :], hv[:, cc, h * 64:(h + 1) * 64]),
                      reads=("ktok", ("hv", par)), writes=(PSR(BA),))
            for h in range(4):
                T.add(PE, o_mm(ps[BB][0:64, h * 64:(h + 1) * 64], hv[:, cc, h * 64:(h + 1) * 64], scT[:, h, :],
                               start=True, stop=False), reads=(("hv", par), "scT"), writes=(PSR(BB),))
                T.add(PE, o_mm(ps[BB][0:64, h * 64:(h + 1) * 64], Stl[:, h, :], qtl[:, h, cs],
                               start=False, stop=True), reads=("Stl", "qtl"), writes=(PSR(BB),))
            T.add(DVE, o_tt(Up[:], ps[BA][0:64, 0:256].rearrange("p (h t) -> p h t", h=4),
                            bc(eLm[:, :, cc], 2, [64, 4, 64]), ALU.mult), reads=(PSR(BA), "eLm"), writes=("Up",))
            T.add(DVE, o_tt(Sst[:], Sst[:], bc(eL[:, :, cc], 2, [64, 4, 64]), ALU.mult), reads=("Sst", "eL"),
                  writes=("Sst",))
            T.add(DVE, o_tt(Sst[:], Sst[:], Up[:], ALU.add), reads=("Sst", "Up"), writes=("Sst",))
            src = ps[BB][0:64, 0:256].rearrange("p (h t) -> p h t", h=4)
            T.add(ACT, o_act(oT[:, :, cs], src, AF.Copy), reads=(PSR(BB),), writes=("hC",))
            T.add(ACT, o_act(sqo[:, :, cs], src, AF.Square), reads=(PSR(BB),), writes=("hB",))
            yield
        for hp in range(2):
            b = BA if hp == 0 else BB
            for k2 in range(2):
                T.add(PE, o_mm(ps[b][0:64, k2 * TA:(k2 + 1) * TA], onesb[0:64, 0:64], sqo[:, 2 * hp + k2, :]),
                      reads=("hB", "onesb"), writes=(PSR(b),))
            T.add(ACT, o_act(rso[:, 2 * hp:2 * hp + 2, :], ps[b][0:64, 0:2 * TA].rearrange("p (c t) -> p c t", c=2),
                             AF.Ln, bias=EPS, scale=1.0 / 64), reads=(PSR(b),), writes=("hE",))
        yield
        T.add(ACT, o_act(rso[:], rso[:], AF.Exp, scale=-0.5), reads=("hE",), writes=("hE",))
        T.add(DVE, o_tt(oT[:], oT[:], rso[:], ALU.mult), reads=("hC", "hE"), writes=("hC",))
        T.add(DVE, o_tt(yhT[:], oT[:], gs[:], ALU.mult), reads=("hC", ("gs", par)), writes=("yhT",))
        if j == 0:
            tap("yhT", yhT[:], "yhT")
        yield

    def attn(j):
        par = j % 2
        aq, sqa, ak, sqk = aq2[par], sqa2[par], ak2[par], sqk2[par]
        for qp in range(2):
            b = 5 + qp
            for k2 in range(2):
                T.add(PE, o_mm(ps[b][:, k2 * TA:(k2 + 1) * TA], bonesb[:], sqa[:, 2 * qp + k2, :]),
                      reads=(("sqa", par), "bonesb"), writes=(PSR(b),))
            T.add(ACT, o_act(rq[:, 2 * qp:2 * qp + 2, :], ps[b][:, 0:2 * TA].rearrange("p (c t) -> p c t", c=2),
                             AF.Ln, bias=EPS, scale=1.0 / 64), reads=(PSR(b),), writes=("rq",))
        T.add(PE, o_mm(ps[7][:, 0:TA], bonesb[:], sqk[:]), reads=(("sqk", par), "bonesb"), writes=(PSR(7),))
        T.add(ACT, o_act(rk[:], ps[7][:, 0:TA], AF.Ln, bias=EPS, scale=1.0 / 64), reads=(PSR(7),), writes=("rk",))
        yield
        T.add(ACT, o_act(rq[:], rq[:], AF.Exp, scale=-0.5), reads=("rq",), writes=("rq",))
        T.add(ACT, o_act(rk[:], rk[:], AF.Exp, scale=-0.5), reads=("rk",), writes=("rk",))
        yield
        aslot0 = (j * NBK) % 4
        T.add(DVE, o_stt(aqn[:].rearrange("p c t -> p (c t)"), aq[:].rearrange("p c t -> p (c t)"),
                         qnw[:, l:l + 1], rq[:].rearrange("p c t -> p (c t)"), ALU.mult, ALU.mult),
              reads=(("aq", par), "rq", "qnw"), writes=("aqn",))
        T.add(DVE, o_stt(akn[:, aslot0:aslot0 + NBK, :].rearrange("p b t -> p (b t)"), ak[:], knw[:, l:l + 1], rk[:],
                         ALU.mult, ALU.mult), reads=(("ak", par), "rk", "knw"), writes=("akn",))
        yield
        for bk in range(NBK):
            gblk = j * NBK + bk
            rels = [(1, gblk)] if gblk == 0 else [(0, gblk - 1), (1, gblk)]
            qsl = slice(bk * 128, (bk + 1) * 128)
            ui = 0
            for rel, kb in rels:
                for hg in range(2):
                    b = 5 + (ui % 2)
                    px = pex[ui % 2]
                    PXR = ("pex", ui % 2)
                    ui += 1
                    for p in range(4):
                        T.add(PE, o_mm(ps[b][:, p * 128:(p + 1) * 128], akn[hg * 64:(hg + 1) * 64, kb % 4, :],
                                       aqn[hg * 64:(hg + 1) * 64, p, qsl]), reads=("akn", "aqn"), writes=(PSR(b),))
                    T.add(ACT, o_act(px[:], ps[b][:, :], AF.Exp, scale=0.125), reads=(PSR(b),), writes=(PXR,))
                    T.add(DVE, o_tt(pT[:, rel, hg * 4:hg * 4 + 4, :].rearrange("p h q -> p (h q)"), px[:],
                                    emask[:, rel, hg * 4:hg * 4 + 4, :].rearrange("p h q -> p (h q)"), ALU.mult),
                          reads=(PXR, "emask"), writes=("pT",))
                    yield
            for hb in range(2):
                b = 7 if hb == 0 else 5
                for hh in range(4):
                    h = hb * 4 + hh
                    for ri, (rel, kb) in enumerate(rels):
                        T.add(PE, o_mm(ps[b][:, hh * 128:hh * 128 + 65], pT[:, rel, h, :], av[:, kb % NAV, hb, :],
                                       start=(ri == 0), stop=(ri == len(rels) - 1)), reads=("pT", "av"),
                              writes=(PSR(b),))
                pv = ps[b][:, :].rearrange("p (h d) -> p h d", h=4)
                T.add(DVE, o_tt(den[:, hb * 4:hb * 4 + 4], pv[:, :, 64], esink[:, l, hb * 4:hb * 4 + 4], ALU.add),
                      reads=(PSR(b), "esink"), writes=("den",))
                T.add(DVE, o_recip(den[:, hb * 4:hb * 4 + 4], den[:, hb * 4:hb * 4 + 4]), reads=("den",), writes=("den",))
                T.add(DVE, o_tt(ytok[:, hb * 4:hb * 4 + 4, :], pv[:, :, 0:64],
                                bc(den[:, hb * 4:hb * 4 + 4], 2, [128, 4, 64]), ALU.mult),
                      reads=(PSR(b), "den"), writes=("ytok",))
                yield
            ps6b = ps[6][:, 0:256].bitcast(BF16)
            ytf = ytok[:].rearrange("p h d -> p (h d)")
            for kc in range(4):
                T.add(PE, o_tr(ps6b[:, kc * 128:(kc + 1) * 128], ytf[:, kc * 128:(kc + 1) * 128], identb[:]),
                      reads=("ytok", "identb"), writes=(PSR(6),))
            T.add(ACT, o_act(yaT[:, :, qsl], ps6b.rearrange("p (c t) -> p c t", c=4), AF.Copy), reads=(PSR(6),),
                  writes=("yaT",))
            yield
        if j == 0:
            tap("yaT", yaT[:], "yaT")

    def wout(j):
        par = j % 2
        xs = xt[par]
        XR = ("xt", par)
        pool_post(j, 2)
        if j == 0:
            tap("ypT", ypT[:], "ypT")
        for dc in range(8):
            b = 3 + (dc // 2) % 4
            off = (dc % 2) * TA
            dsl = slice(dc * 128, (dc + 1) * 128)
            out = ps[b][:, off:off + TA]
            for kc in range(2):
                T.add(PE, o_mm(out, wop[:, kc, dsl], ypT[:, kc, :], start=(kc == 0), stop=False),
                      reads=("wop", "ypT"), writes=(PSR(b),))
            for h in range(4):
                T.add(PE, o_mm(out, woh[:, h, dsl], yhT[:, h, :], start=False, stop=False),
                      reads=("woh", "yhT"), writes=(PSR(b),))
            for kc in range(4):
                T.add(PE, o_mm(out, woa[:, kc, dsl], yaT[:, kc, :], start=False, stop=(kc == 3)),
                      reads=("woa", "yaT"), writes=(PSR(b),))
            T.add(DVE, o_stt(tmp[:, dc, :], out, g1[:, dc:dc + 1], xs[:, dc, :], ALU.mult, ALU.add),
                  reads=(PSR(b), XR, "P6"), writes=("tmp",))
        T.add(SP, o_dma(ydv[:, :, j * TA:(j + 1) * TA], tmp[:]), reads=("tmp",), writes=(("y", j),), chan="yst")

    def limited(g, n):
        for _ in range(n):
            try:
                next(g)
            except StopIteration:
                return
            yield

    def delayed(g, k):
        for _ in range(k):
            yield
        yield from g

    load_x(0)
    if ntiles > 1:
        load_x(1)
    drain(front(0))
    for j in range(ntiles):
        pool_pre(j)
        gf = front(j + 1) if j + 1 < ntiles else None
        interleave([hgrn(j), attn(j), delayed(limited(gf, 2), 6) if gf is not None else None])
        if gf is not None:
            drain(gf)
        wout(j)
        if j + 2 < ntiles:
            load_x(j + 2)
    arena.reset(mark)


def phase_b(nc, arena, T, ps, PSR, l, y_d, G, tap, load):
    identb, onesb, sel2, P6, rw, rb = G["identb"], G["onesb"], G["sel2"], G["P6"], G["rw"], G["rb"]
    mark = arena.mark()

    def sb(name, shape, dt=F32):
        return arena.alloc(list(shape), dt)

    yacc = sb("yacc", [128, 8, TB])
    h2T = sb("h2T", [128, 8, TB], BF16)
    Wg = [sb("Wg%d" % i, [128, 8, 512], BF16) for i in range(2)]
    Wu = [sb("Wu%d" % i, [128, 8, 512], BF16) for i in range(2)]
    Wd = [sb("Wd%d" % i, [128, 4, D], BF16) for i in range(2)]
    Hb = [sb("Hb%d" % i, [128, 4, SUB], BF16) for i in range(2)]
    sqb = sb("sqb", [128, 8, SUB], BF16)
    tmpb = [sb("tmpb%d" % i, [128, SUB]) for i in range(2)]
    sg = [sb("sg%d" % i, [128, SUB]) for i in range(2)]
    t1 = [sb("t1%d" % i, [128, SUB]) for i in range(2)]
    gbc = [sb("gbc%d" % i, [128, SUB]) for i in range(2)]
    gT2 = sb("gT2", [48, TB], BF16)
    rstdb = sb("rstdb", [128, SUB])
    Wp = sb("Wp", [128, 8, NE])
    B2bc = sb("B2bc", [128, 8, 128])
    bpr = sb("bpr", [128, NE])
    NB = SUB // 128
    rtok = sb("rtok", [128, NB])
    lg = sb("lg", [128, NB, NE])
    sc = sb("sc", [128, NB, NE])
    se = sb("se", [128, NB, NE])
    se2 = sb("se2", [128, NB, NE])
    ch = sb("ch", [128, NB, NE])
    r1 = sb("r1", [128, NB])
    m1 = sb("m1", [128, NB, 4])
    m2 = sb("m2", [128, NB, 4])
    gsc = sb("gsc", [128, NB, 4])
    gts = sb("gts", [128, NB, NE])
    g3 = sb("g3", [128, NB, 48], BF16)

    A2 = P6[:, l, 3, :]
    B2 = P6[:, l, 4, :]
    g2 = P6[:, l, 5, :]
    RB = 7
    T.add(DVE, o_tt(Wp[:], rw[:], bc(A2, 2, [128, 8, NE]), ALU.mult), reads=("rw", "P6"), writes=("Wp",))
    T.add(DVE, o_copy(B2bc[:], bc(B2, 2, [128, 8, 128])), reads=("P6",), writes=("B2bc",))
    for c in range(8):
        T.add(PE, o_mm(ps[RB][:, 0:NE], B2bc[:, c, :], rw[:, c, :], start=(c == 0), stop=(c == 7)),
              reads=("B2bc", "rw"), writes=(PSR(RB),))
    T.add(DVE, o_copy(bpr[:], ps[RB][:, 0:NE]), reads=(PSR(RB),), writes=("bpr",))
    T.add(DVE, o_memset(g3[:], 0.0), writes=("g3",))

    ydv = y_d.rearrange("(c p) t -> p c t", p=128)

    def load_expert(e):
        k = e % 2
        load(Wg[k][:], G["wg_d"][l, e], ("Wg", k), eng=POOL)
        load(Wu[k][:], G["wu_d"][l, e], ("Wu", k), eng=POOL)
        load(Wd[k][:], G["wd_d"][l, e], ("Wd", k), eng=POOL)

    def norm_route(s_):
        ss = slice(s_ * SUB, (s_ + 1) * SUB)
        YR = ("yacc", s_)
        T.add(ACT, o_act(sqb[:], yacc[:, :, ss], AF.Square), reads=(YR,), writes=("sqb",))
        for c in range(8):
            T.add(PE, o_mm(ps[RB][:, :], onesb[:], sqb[:, c, :], start=(c == 0), stop=(c == 7)),
                  reads=("sqb", "onesb"), writes=(PSR(RB),))
        T.add(ACT, o_act(rstdb[:], ps[RB][:, :], AF.Ln, bias=EPS, scale=1.0 / D), reads=(PSR(RB),), writes=("rstdb",))
        T.add(ACT, o_act(rstdb[:], rstdb[:], AF.Exp, scale=-0.5), reads=("rstdb",), writes=("rstdb",))
        yield
        for c in range(8):
            tb = tmpb[c % 2]
            TR = ("tmpb", c % 2)
            T.add(DVE, o_tt(tb[:], yacc[:, c, ss], rstdb[:], ALU.mult), reads=(YR, "rstdb"), writes=(TR,))
            T.add(POOL, o_ts(h2T[:, c, ss], tb[:], A2[:, c:c + 1], ALU.mult, B2[:, c:c + 1], ALU.add),
                  reads=(TR, "P6"), writes=(("h2T", s_),))
            if c % 2 == 1:
                yield
        for bk in range(NB):
            tsl = slice(s_ * SUB + bk * 128, s_ * SUB + (bk + 1) * 128)
            for c in range(8):
                T.add(PE, o_mm(ps[RB][:, bk * NE:(bk + 1) * NE], yacc[:, c, tsl], Wp[:, c, :],
                               start=(c == 0), stop=(c == 7)), reads=(YR, "Wp"), writes=(PSR(RB),))
            for c in range(8):
                T.add(PE, o_mm(ps[RB][:, 64 + bk:65 + bk], sqb[:, c, bk * 128:(bk + 1) * 128], onesb[:, 0:1],
                               start=(c == 0), stop=(c == 7)), reads=("sqb", "onesb"), writes=(PSR(RB),))
            yield
        T.add(ACT, o_act(rtok[:], ps[RB][:, 64:64 + NB], AF.Ln, bias=EPS, scale=1.0 / D), reads=(PSR(RB),),
              writes=("rtok",))
        T.add(ACT, o_act(rtok[:], rtok[:], AF.Exp, scale=-0.5), reads=("rtok",), writes=("rtok",))
        L3 = ps[RB][:, 0:NB * NE].rearrange("p (b e) -> p b e", e=NE)
        T.add(DVE, o_tt(lg[:], L3, bc(rtok[:], 2, [128, NB, NE]), ALU.mult), reads=(PSR(RB), "rtok"), writes=("lg",))
        T.add(DVE, o_tt(lg[:], lg[:], bc(bpr[:], 1, [128, NB, NE]), ALU.add), reads=("lg", "bpr"), writes=("lg",))
        T.add(DVE, o_red(r1[:], lg[:], ALU.max), reads=("lg",), writes=("r1",))
        T.add(DVE, o_tt(lg[:], lg[:], bc(r1[:], 2, [128, NB, NE]), ALU.subtract), reads=("lg", "r1"), writes=("lg",))
        yield
        T.add(ACT, o_act(sc[:], lg[:], AF.Exp), reads=("lg",), writes=("sc",))
        T.add(DVE, o_red(r1[:], sc[:], ALU.add), reads=("sc",), writes=("r1",))
        T.add(DVE, o_recip(r1[:], r1[:]), reads=("r1",), writes=("r1",))
        T.add(DVE, o_tt(sc[:], sc[:], bc(r1[:], 2, [128, NB, NE]), ALU.mult), reads=("sc", "r1"), writes=("sc",))
        T.add(DVE, o_tt(se[:], sc[:], bc(rb[:], 1, [128, NB, NE]), ALU.add), reads=("sc", "rb"), writes=("se",))
        yield
        se4 = se[:].rearrange("p b (g k) -> p b g k", k=4)
        se24 = se2[:].rearrange("p b (g k) -> p b g k", k=4)
        ch4 = ch[:].rearrange("p b (g k) -> p b g k", k=4)
        T.add(DVE, o_red(m1[:], se4, ALU.max), reads=("se",), writes=("m1",))
        T.add(DVE, o_tt(ch4, se4, bc(m1[:], 3, [128, NB, 4, 4]), ALU.is_equal), reads=("se", "m1"), writes=("ch",))
        T.add(DVE, o_stt(se2[:].rearrange("p b e -> p (b e)"), ch[:].rearrange("p b e -> p (b e)"), -1e9,
                         se[:].rearrange("p b e -> p (b e)"), ALU.mult, ALU.add), reads=("ch", "se"), writes=("se2",))
        T.add(DVE, o_red(m2[:], se24, ALU.max), reads=("se2",), writes=("m2",))
        yield
        T.add(DVE, o_tt(gsc[:], m1[:], m2[:], ALU.add), reads=("m1", "m2"), writes=("gsc",))
        T.add(DVE, o_red(r1[:], gsc[:], ALU.max), reads=("gsc",), writes=("r1",))
        T.add(DVE, o_tt(gsc[:], gsc[:], bc(r1[:], 2, [128, NB, 4]), ALU.is_equal), reads=("gsc", "r1"), writes=("gsc",))
        T.add(DVE, o_tt(ch4, se4, bc(m2[:], 3, [128, NB, 4, 4]), ALU.is_ge), reads=("se", "m2"), writes=("ch",))
        T.add(DVE, o_tt(ch4, ch4, bc(gsc[:], 3, [128, NB, 4, 4]), ALU.mult), reads=("ch", "gsc"), writes=("ch",))
        yield
        T.add(DVE, o_tt(gts[:], sc[:], ch[:], ALU.mult), reads=("sc", "ch"), writes=("gts",))
        T.add(DVE, o_red(r1[:], gts[:], ALU.add), reads=("gts",), writes=("r1",))
        T.add(DVE, o_recip(r1[:], r1[:]), reads=("r1",), writes=("r1",))
        T.add(DVE, o_tt(gts[:], gts[:], bc(r1[:], 2, [128, NB, NE]), ALU.mult), reads=("gts", "r1"), writes=("gts",))
        T.add(DVE, o_copy(g3[:, :, 0:NE], gts[:]), reads=("gts",), writes=("g3",))
        T.add(DVE, o_tt(g3[:, :, 32:48], gts[:], g3[:, :, 0:NE], ALU.subtract), reads=("gts", "g3"), writes=("g3",))
        yield
        psb = ps[RB][0:48, 0:256].bitcast(BF16)
        for bk in range(NB):
            T.add(PE, o_tr(psb[:, bk * 128:(bk + 1) * 128], g3[:, bk, :], identb[:]), reads=("g3", "identb"),
                  writes=(PSR(RB),))
        T.add(ACT, o_act(gT2[:, ss], psb, AF.Copy), reads=(PSR(RB),), writes=(("gT2", s_),))
        yield

    items = [(e, s_) for e in range(NE) for s_ in range(NSUB)]

    def emit_D(n):
        e, s_ = items[n]
        k = e % 2
        hb = Hb[n % 2]
        HR = ("Hb", n % 2)
        ss = slice(s_ * SUB, (s_ + 1) * SUB)
        for dc in range(8):
            b = 5 + dc % 2
            for jc in range(4):
                T.add(PE, o_mm(ps[b][:, :], Wd[k][:, jc, dc * 128:(dc + 1) * 128], hb[:, jc, :],
                               start=(jc == 0), stop=(jc == 3)), reads=(("Wd", k), HR), writes=(PSR(b),))
            T.add(DVE, o_stt(yacc[:, dc, ss], ps[b][:, :], g2[:, dc:dc + 1], yacc[:, dc, ss], ALU.mult, ALU.add),
                  reads=(PSR(b), ("yacc", s_), "P6"), writes=(("yacc", s_),))
            if dc % 2 == 1:
                yield

    def emit_GU(n):
        e, s_ = items[n]
        k = e % 2
        hb = Hb[n % 2]
        HR = ("Hb", n % 2)
        ss = slice(s_ * SUB, (s_ + 1) * SUB)
        gb_ = gbc[n % 2]
        GR = ("gbc", n % 2)
        T.add(PE, o_mm(ps[4][:, :], sel2[:, e, :], gT2[:, ss]), reads=("sel2", ("gT2", s_)), writes=(PSR(4),))
        T.add(ACT, o_act(gb_[:], ps[4][:, :], AF.Copy), reads=(PSR(4),), writes=(GR,))
        for jc in range(4):
            bg = jc % 2
            bu = 2 + jc % 2
            for c in range(8):
                T.add(PE, o_mm(ps[bg][:, :], Wg[k][:, c, jc * 128:(jc + 1) * 128], h2T[:, c, ss],
                               start=(c == 0), stop=(c == 7)), reads=(("Wg", k), ("h2T", s_)), writes=(PSR(bg),))
            for c in range(8):
                T.add(PE, o_mm(ps[bu][:, :], Wu[k][:, c, jc * 128:(jc + 1) * 128], h2T[:, c, ss],
                               start=(c == 0), stop=(c == 7)), reads=(("Wu", k), ("h2T", s_)), writes=(PSR(bu),))
            T.add(ACT, o_act(sg[jc % 2][:], ps[bg][:, :], AF.Silu), reads=(PSR(bg),), writes=(("sg", jc % 2),))
            T.add(DVE, o_tt(t1[jc % 2][:], ps[bu][:, :], sg[jc % 2][:], ALU.mult),
                  reads=(PSR(bu), ("sg", jc % 2)), writes=(("t1", jc % 2),))
            T.add(DVE, o_tt(hb[:, jc, :], t1[jc % 2][:], gb_[:], ALU.mult), reads=(("t1", jc % 2), GR), writes=(HR,))
            yield

    def item(n):
        yield from emit_GU(n)
        if n >= 1:
            yield from emit_D(n - 1)

    def store_y(J, s_):
        T0 = J * TB
        T.add(SP, o_dma(ydv[:, :, T0 + s_ * SUB:T0 + (s_ + 1) * SUB], yacc[:, :, s_ * SUB:(s_ + 1) * SUB]),
              reads=(("yacc", s_),), writes=(("yB", J, s_),), chan="ystb%d" % s_)

    def load_y(J, s_):
        T0 = J * TB
        load(yacc[:, :, s_ * SUB:(s_ + 1) * SUB], ydv[:, :, T0 + s_ * SUB:T0 + (s_ + 1) * SUB], ("yacc", s_))

    for s_ in range(NSUB):
        load_y(0, s_)
    load_expert(0)
    drain(norm_route(0))
    for J in range(NTB):
        last = (J + 1 == NTB)
        for n, (e, s_) in enumerate(items):
            gens = [item(n)]
            if e == 0 and s_ + 1 < NSUB:
                gens.append(norm_route(s_ + 1))
            if e == NE - 1 and s_ == 2 and not last:
                gens.append(norm_route(0))
            interleave(gens)
            if s_ == 0 and e + 1 < NE:
                load_expert(e + 1)
            if e == NE - 1:
                if s_ >= 1:
                    store_y(J, s_ - 1)
                    if not last:
                        load_y(J + 1, s_ - 1)
                if s_ == 0 and not last:
                    load_expert(0)
        drain(emit_D(len(items) - 1))
        store_y(J, NSUB - 1)
        if not last:
            load_y(J + 1, NSUB - 1)
    arena.reset(mark)


def _consts():
    ident = np.eye(128, dtype=np.float32)
    slopes = np.exp2(-8.0 * np.arange(1, 9, dtype=np.float32) / 8).astype(np.float32)
    k = np.arange(128)[:, None]
    q = np.arange(128)[None, :]
    emask = np.zeros((128, 2, 8, 128), np.float32)
    for h in range(8):
        d_prev = (q + 128 - k).astype(np.float32)
        emask[:, 0, h, :] = np.where(k > q, np.exp(-slopes[h] * d_prev), 0.0)
        d_own = (q - k).astype(np.float32)
        emask[:, 1, h, :] = np.where(q >= k, np.exp(-slopes[h] * d_own), 0.0)
    s = np.arange(64)[:, None]
    t = np.arange(64)[None, :]
    cmask = (s <= t).astype(np.float32)
    sel = np.zeros((48, NE, 128), np.float32)
    for e in range(NE):
        sel[e, e, :] = 1.0
        sel[32 + e, e, :] = 1.0
    cinv = np.zeros((128, 2, 2, TA), np.float32)
    wins = (2, 4, 8, 16)
    pos = np.arange(TA, dtype=np.float32)
    for cp in range(2):
        for half in range(2):
            w = wins[2 * cp + half]
            rows = slice(half * 64, (half + 1) * 64)
            cinv[rows, 0, cp, :] = 1.0 / np.minimum(pos + 1.0, float(w))
            cinv[rows, 1, cp, :] = 1.0 / float(w)
    bones = np.zeros((128, 128), np.float32)
    bones[0:64, 0:64] = 1.0
    bones[64:128, 64:128] = 1.0
    invw = np.ascontiguousarray(cinv[:, 1, :, 0])
    return dict(ident=ident, emask=emask, cmask=cmask, sel=sel, cinv=cinv, bones=bones, invw=invw)


def prep_shared(inp):
    f = lambda a: np.ascontiguousarray(np.asarray(a, dtype=np.float32))
    sh = {}
    sh["ada_w"] = f(inp["ada_w"])
    sh["ada_b"] = f(np.asarray(inp["ada_b"]).reshape(DEPTH, 48, 128).transpose(2, 0, 1))
    sh["n1w"] = f(np.asarray(inp["norm1_w"]).reshape(DEPTH, 8, 128).transpose(2, 0, 1))
    sh["n2w"] = f(np.asarray(inp["norm2_w"]).reshape(DEPTH, 8, 128).transpose(2, 0, 1))
    w_in = np.asarray(inp["w_in"], dtype=np.float32)
    cols = np.arange(2048)
    aq = 1280 + np.array([[(p + 4 * half) * 64 + d for half in range(2) for d in range(64)] for p in range(4)]).reshape(-1)
    cols[1280:1792] = aq
    w_in = w_in[:, :, cols]
    sh["w_in"] = f(w_in.reshape(DEPTH, 8, 128, 2048).transpose(0, 2, 1, 3))
    pw = np.asarray(inp["pool_w"], dtype=np.float32)
    pwbd = np.zeros((128, DEPTH, 2, 128), np.float32)
    for l in range(DEPTH):
        for cp in range(2):
            pwbd[0:64, l, cp, 0:64] = pw[l, 2 * cp]
            pwbd[64:128, l, cp, 64:128] = pw[l, 2 * cp + 1]
    sh["pwbd"] = pwbd
    sh["pscale"] = f(np.asarray(inp["pool_scale"]).reshape(DEPTH, 2, 128).transpose(2, 0, 1))
    sh["lbraw"] = f(np.asarray(inp["hgrn_lb_raw"]).reshape(DEPTH, 4, 64).transpose(2, 0, 1))
    sh["hnw"] = f(np.asarray(inp["hgrn_norm_w"]).reshape(DEPTH, 4, 64).transpose(2, 0, 1))
    sh["qnw"] = f(np.tile(np.asarray(inp["q_norm_w"]).T, (2, 1)))
    sh["knw"] = f(np.tile(np.asarray(inp["k_norm_w"]).T, (2, 1)))
    sh["sinks"] = f(np.broadcast_to(np.asarray(inp["attn_sinks"])[None], (128, DEPTH, 8)))
    wo = np.asarray(inp["w_out"], dtype=np.float32)
    sh["wo_p"] = f(wo[:, 0:256].reshape(DEPTH, 2, 128, D).transpose(0, 2, 1, 3))
    sh["wo_h"] = f(wo[:, 256:512].reshape(DEPTH, 4, 64, D).transpose(0, 2, 1, 3))
    sh["wo_a"] = f(wo[:, 512:1024].reshape(DEPTH, 4, 128, D).transpose(0, 2, 1, 3))
    sh["rw"] = f(np.asarray(inp["router_w"]).reshape(8, 128, NE).transpose(1, 0, 2))
    sh["rb"] = f(np.broadcast_to(np.asarray(inp["router_bias"])[None], (128, NE)))
    sh["wg"] = f(np.asarray(inp["expert_w_gate"]).reshape(DEPTH, NE, 8, 128, 512).transpose(0, 1, 3, 2, 4))
    sh["wu"] = f(np.asarray(inp["expert_w_up"]).reshape(DEPTH, NE, 8, 128, 512).transpose(0, 1, 3, 2, 4))
    sh["wd"] = f(np.asarray(inp["expert_w_down"]).reshape(DEPTH, NE, 4, 128, D).transpose(0, 1, 3, 2, 4))
    sh.update(_consts())
    return sh


def prep_core(inp, b):
    x = np.asarray(inp["x"][b], dtype=np.float32)
    c = np.asarray(inp["c"][b], dtype=np.float32)
    return {"xT": np.ascontiguousarray(x.T), "c_in": np.ascontiguousarray(c.reshape(8, 128).T)}


_CACHE = {}


def kernel(**inputs):
    if "nc" not in _CACHE:
        _CACHE["nc"] = build_program()[0]
    nc = _CACHE["nc"]
    sh = prep_shared(inputs)
    B = np.asarray(inputs["x"]).shape[0]
    in_maps = []
    for b in range(B):
        m = dict(sh)
        m.update(prep_core(inputs, b))
        in_maps.append(m)
    res = run_bass_kernel_spmd(nc, in_maps, core_ids=list(range(B)))
    out = np.stack([np.ascontiguousarray(res.results[b]["y"].T) for b in range(B)], axis=0)
    return out.astype(np.float32)
```

```python
import numpy as np
from contextlib import ExitStack

import concourse.bass as bass
import concourse.mybir as mybir
from concourse.bass_utils import run_bass_kernel_spmd

F32 = mybir.dt.float32
BF16 = mybir.dt.bfloat16
AF = mybir.ActivationFunctionType
ALU = mybir.AluOpType
AX = mybir.AxisListType

PE, ACT, DVE, POOL, SP = "pe", "act", "dve", "pool", "sp"
ENGS = (PE, ACT, DVE, POOL, SP)

D = 1024
S = 4096
DEPTH = 4
NE = 16
EPS = 1e-6
MAXK = 1.0 - 1e-6
TA = 256
NTA = S // TA
TB = 2048
NTB = S // TB
SUB = 512
ARENA_WORDS = 52992
NSUB = TB // SUB


class Tracker:
    def __init__(self):
        self.ops = []
        self.lastw = {}
        self.rds = {}
        self.chan_count = {}
        self.last_of_eng = {}
        self.last_of_chan = {}
        self.pending = {}

    def add(self, eng, fn, reads=(), writes=(), chan=None):
        i = len(self.ops)
        deps = {}

        def dep(j, kind):
            if j is None or j == i:
                return
            if deps.get(j) == "RAW":
                return
            deps[j] = kind

        for r in reads:
            dep(self.lastw.get(r), "RAW")
        for w in writes:
            dep(self.lastw.get(w), "WAW")
            for rd in self.rds.get(w, {}).values():
                dep(rd, "WAR")
        for j, k in self.pending.pop(eng, {}).items():
            dep(j, k)
        op = dict(eng=eng, fn=fn, deps=deps, chan=chan)
        if chan is not None:
            n = self.chan_count.get(chan, 0) + 1
            self.chan_count[chan] = n
            op["chan_idx"] = n
            self.last_of_chan[chan] = i
        self.last_of_eng[eng] = i
        self.ops.append(op)
        rkey = eng if chan is None else ("chan", chan)
        for r in reads:
            self.rds.setdefault(r, {})[rkey] = i
        for w in writes:
            self.lastw[w] = i
            self.rds[w] = {}
        return i

    def barrier(self):
        b = {}
        for e, j in self.last_of_eng.items():
            b[j] = "RAW"
        for c, j in self.last_of_chan.items():
            b[j] = "RAW"
        for e in ENGS:
            d = dict(self.pending.get(e, {}))
            d.update(b)
            self.pending[e] = d

    def emit(self, nc, stack):
        ops = self.ops
        need = [False] * len(ops)

        def needs_wait(op, pj, kind):
            if pj["chan"] is not None:
                return True
            if op["chan"] is not None:
                return True
            if pj["eng"] != op["eng"]:
                return True
            if op["eng"] == PE:
                return False
            return True

        for op in ops:
            for j, kind in op["deps"].items():
                if needs_wait(op, ops[j], kind) and ops[j]["chan"] is None:
                    need[j] = True
        cnt = {e: 0 for e in ENGS}
        for i, op in enumerate(ops):
            if op["chan"] is None and need[i]:
                cnt[op["eng"]] += 1
                op["sig"] = cnt[op["eng"]]
        esem = {e: stack.enter_context(nc.semaphore("es_" + e)) for e in ENGS}
        csem = {c: stack.enter_context(nc.semaphore("cs_%d" % k)) for k, c in enumerate(self.chan_count)}
        per_eng = {e: [] for e in ENGS}
        for i, op in enumerate(ops):
            per_eng[op["eng"]].append(i)

        def run(eng_name, eng):
            waited = {}
            for i in per_eng[eng_name]:
                op = ops[i]
                for j, kind in op["deps"].items():
                    pj = ops[j]
                    if not needs_wait(op, pj, kind):
                        continue
                    if pj["chan"] is not None:
                        key, val, sem = ("c", pj["chan"]), 16 * pj["chan_idx"], csem[pj["chan"]]
                    else:
                        key, val, sem = ("e", pj["eng"]), pj["sig"], esem[pj["eng"]]
                    if waited.get(key, 0) >= val:
                        continue
                    waited[key] = val
                    eng.wait_ge(sem, val)
                ins = op["fn"](eng)
                if op["chan"] is not None:
                    ins.then_inc(csem[op["chan"]], 16)
                elif need[i]:
                    ins.then_inc(esem[eng_name], 1)
            if eng_name == SP:
                for c, n in self.chan_count.items():
                    if waited.get(("c", c), 0) < 16 * n:
                        eng.wait_ge(csem[c], 16 * n)
                for e in ENGS:
                    if cnt[e] > 0 and waited.get(("e", e), 0) < cnt[e]:
                        eng.wait_ge(esem[e], cnt[e])

        with nc.Block() as block:
            block.tensor(lambda e: run(PE, e))
            block.scalar(lambda e: run(ACT, e))
            block.vector(lambda e: run(DVE, e))
            block.gpsimd(lambda e: run(POOL, e))
            block.sync(lambda e: run(SP, e))
        return cnt


class Arena:
    def __init__(self, nc, stack, words):
        self.t = stack.enter_context(nc.sbuf_tensor("arena", [128, words], F32))
        self.words = words
        self.off = 0
        self.peak = 0

    def mark(self):
        return self.off

    def reset(self, m):
        self.off = m

    def alloc(self, shape, dt=F32, at=None):
        if at is not None:
            save = self.off
            self.off = at
            ap = self.alloc(shape, dt)
            self.off = save
            return ap
        P = shape[0]
        n = 1
        for s_ in shape[1:]:
            n *= s_
        words = n if dt == F32 else (n + 1) // 2
        words = (words + 1) // 2 * 2
        assert self.off + words <= self.words, ("SBUF arena overflow", self.off, words, self.words)
        ap = self.t[0:P, self.off:self.off + words]
        self.off += words
        self.peak = max(self.peak, self.off)
        if dt != F32:
            ap = ap.bitcast(dt)
        ap = ap[:, 0:n]
        if len(shape) > 2:
            names = ["d%d" % i for i in range(len(shape) - 1)]
            kw = {names[i]: shape[i + 1] for i in range(len(shape) - 1)}
            ap = ap.rearrange("p (%s) -> p %s" % (" ".join(names), " ".join(names)), **kw)
        return ap

def o_mm(out, lhsT, rhs, start=True, stop=True):
    return lambda e: e.matmul(out, lhsT, rhs, start=start, stop=stop)


def o_tr(out, in_, ident):
    return lambda e: e.transpose(out, in_, ident)


def o_act(out, in_, func, bias=None, scale=None):
    kw = {}
    if bias is not None:
        kw["bias"] = bias
    if scale is not None:
        kw["scale"] = scale
    return lambda e: e.activation(out=out, in_=in_, func=func, **kw)


def o_tt(out, in0, in1, op):
    return lambda e: e.tensor_tensor(out=out, in0=in0, in1=in1, op=op)


def o_ts(out, in0, s1, op0, s2=None, op1=None):
    if op1 is None:
        return lambda e: e.tensor_scalar(out=out, in0=in0, scalar1=s1, scalar2=None, op0=op0)
    return lambda e: e.tensor_scalar(out=out, in0=in0, scalar1=s1, scalar2=s2, op0=op0, op1=op1)


def o_stt(out, in0, scalar, in1, op0, op1):
    return lambda e: e.scalar_tensor_tensor(out=out, in0=in0, scalar=scalar, in1=in1, op0=op0, op1=op1)


def o_copy(out, in_):
    return lambda e: e.tensor_copy(out=out, in_=in_)


def o_recip(out, in_):
    return lambda e: e.reciprocal(out=out, in_=in_)


def o_red(out, in_, op):
    return lambda e: e.tensor_reduce(out=out, in_=in_, axis=AX.X, op=op)


def o_memset(ap, v):
    return lambda e: e.memset(ap, v)


def o_dma(out, in_):
    return lambda e: e.dma_start(out=out, in_=in_)


def o_scan(out, d0, d1, init, op0, op1):
    return lambda e: e.tensor_tensor_scan(out=out, data0=d0, data1=d1, initial=init, op0=op0, op1=op1)


def bc(ap, axis, shape):
    return ap.unsqueeze(axis).broadcast_to(list(shape))


def build_program(n_layers=DEPTH, do_b=True, taps=(), stop_a_tiles=None):
    nc = bass.Bass("TRN2", target_bir_lowering=False)
    T = Tracker()
    st = ExitStack()

    def din(name, shape):
        return nc.dram_tensor(name, list(shape), F32, kind="ExternalInput").ap()

    xT_d = din("xT", [D, S])
    c_d = din("c_in", [128, 8])
    adaw_d = din("ada_w", [DEPTH, D, 6 * D])
    adab_d = din("ada_b", [128, DEPTH, 48])
    n1w_d = din("n1w", [128, DEPTH, 8])
    n2w_d = din("n2w", [128, DEPTH, 8])
    win_d = din("w_in", [DEPTH, 128, 8, 2048])
    pwbd_d = din("pwbd", [128, DEPTH, 2, 128])
    pscale_d = din("pscale", [128, DEPTH, 2])
    lbraw_d = din("lbraw", [64, DEPTH, 4])
    hnw_d = din("hnw", [64, DEPTH, 4])
    qnw_d = din("qnw", [128, DEPTH])
    knw_d = din("knw", [128, DEPTH])
    sinks_d = din("sinks", [128, DEPTH, 8])
    wop_d = din("wo_p", [DEPTH, 128, 2, D])
    woh_d = din("wo_h", [DEPTH, 64, 4, D])
    woa_d = din("wo_a", [DEPTH, 128, 4, D])
    rw_d = din("rw", [128, 8, NE])
    rb_d = din("rb", [128, NE])
    wg_d = din("wg", [DEPTH, NE, 128, 8, 512]) if do_b else None
    wu_d = din("wu", [DEPTH, NE, 128, 8, 512]) if do_b else None
    wd_d = din("wd", [DEPTH, NE, 128, 4, D]) if do_b else None
    ident_d = din("ident", [128, 128])
    emask_d = din("emask", [128, 2, 8, 128])
    cmask_d = din("cmask", [64, 64])
    sel_d = din("sel", [48, NE, 128])
    cinv_d = din("cinv", [128, 2, 2, TA])
    bones_d = din("bones", [128, 128])
    invw_d = din("invw", [128, 2])
    y_d = nc.dram_tensor("y", [D, S], F32, kind="ExternalOutput").ap()
    tap_d = {}
    for name, shape in taps:
        tap_d[name] = nc.dram_tensor("tap_" + name, list(shape), F32, kind="ExternalOutput").ap()

    arena = Arena(nc, st, ARENA_WORDS)

    def sb(name, shape, dt=F32):
        return arena.alloc(list(shape), dt)

    ps = [st.enter_context(nc.psum_tensor("ps%d" % b, [128, 512], F32)) for b in range(8)]

    def PSR(b):
        return ("ps", b)

    identf = sb("identf", [128, 128])
    identb = sb("identb", [128, 128], BF16)
    onesb = sb("onesb", [128, 128], BF16)
    bonesb = sb("bonesb", [128, 128], BF16)
    cmask = sb("cmask", [64, 64])
    sel2 = sb("sel2", [48, NE, 128], BF16)
    P6 = sb("P6", [128, DEPTH, 6, 8])
    modp = sb("modp", [128, DEPTH, 48])
    n1w = sb("n1w", [128, DEPTH, 8])
    n2w = sb("n2w", [128, DEPTH, 8])
    pscale = sb("pscale", [128, DEPTH, 2])
    lbm = sb("lbm", [64, DEPTH, 4])
    hnw = sb("hnw", [64, DEPTH, 4])
    qnw = sb("qnw", [128, DEPTH])
    knw = sb("knw", [128, DEPTH])
    esink = sb("esink", [128, DEPTH, 8])
    rw = sb("rw", [128, 8, NE])
    rb = sb("rb", [128, NE])
    cond = sb("cond", [128, 8])

    ndma = [0]

    def load(dst, src, res, eng=SP):
        T.add(eng, o_dma(dst, src), reads=(), writes=(res,), chan="ld_" + str(res))

    def tap(name, src_ap, res, dst=None):
        if name not in tap_d:
            return
        ndma[0] += 1
        T.add(POOL, o_dma(tap_d[name] if dst is None else dst, src_ap), reads=(res,), writes=(("tap", name),),
              chan="tap%d" % ndma[0])

    load(identf[:], ident_d, "identf")
    load(identb[:], ident_d, "identb", eng=POOL)
    load(bonesb[:], bones_d, "bonesb", eng=POOL)
    load(cmask[:], cmask_d, "cmask")
    load(sel2[:], sel_d, "sel2", eng=POOL)
    load(n1w[:], n1w_d, "n1w")
    load(n2w[:], n2w_d, "n2w")
    load(pscale[:], pscale_d, "pscale")
    load(hnw[:], hnw_d, "hnw")
    load(qnw[:], qnw_d, "qnw")
    load(knw[:], knw_d, "knw")
    load(esink[:], sinks_d, "esink")
    load(rw[:], rw_d, "rw")
    load(rb[:], rb_d, "rb")
    load(cond[:], c_d, "cond")
    T.add(DVE, o_memset(onesb[:], 1.0), writes=("onesb",))
    T.add(ACT, o_act(cond[:], cond[:], AF.Silu), reads=("cond",), writes=("cond",))
    T.add(ACT, o_act(esink[:], esink[:], AF.Exp), reads=("esink",), writes=("esink",))

    if True:
        mark0 = arena.mark()

        def sb0(name, shape, dt=F32):
            return arena.alloc(list(shape), dt)

        adab = sb0("adab", [128, DEPTH, 48])
        adaw = [sb0("adaw%d" % i, [128, 8, 512]) for i in range(4)]
        modrow = sb0("modrow", [1, 6 * D])
        lbr = sb0("lbr", [64, DEPTH, 4])
        lbs = sb0("lbs", [64, 4])
        load(adab[:], adab_d, "adab")
        load(lbr[:], lbraw_d, "lbr")
        nblk = 0
        for l in range(n_layers):
            for nb in range(12):
                k = nblk % 4
                nblk += 1
                load(adaw[k][:], adaw_d[l, :, nb * 512:(nb + 1) * 512].rearrange("(c p) n -> p c n", p=128),
                     ("adaw", k), eng=(SP, POOL, SP, POOL)[k])
                for c in range(8):
                    T.add(PE, o_mm(ps[k][0:1, :], cond[:, c:c + 1], adaw[k][:, c, :], start=(c == 0), stop=(c == 7)),
                          reads=(("adaw", k), "cond"), writes=(PSR(k),))
                T.add(DVE, o_copy(modrow[:, nb * 512:(nb + 1) * 512], ps[k][0:1, :]), reads=(PSR(k),),
                      writes=("modrow",))
            for f in range(48):
                T.add(PE, o_tr(ps[4][:, f:f + 1], modrow[0:1, f * 128:(f + 1) * 128], identf[0:1, 0:1]),
                      reads=("modrow", "identf"), writes=(PSR(4),))
            T.add(DVE, o_tt(modp[:, l, :], ps[4][:, 0:48], adab[:, l, :], ALU.add), reads=(PSR(4), "adab"),
                  writes=("modp",))
            T.add(DVE, o_stt(P6[:, l, 0, :], modp[:, l, 8:16], 1.0, n1w[:, l, :], ALU.add, ALU.mult),
                  reads=("modp", "n1w"), writes=("P6",))
            T.add(DVE, o_copy(P6[:, l, 1, :], modp[:, l, 0:8]), reads=("modp",), writes=("P6",))
            T.add(DVE, o_copy(P6[:, l, 2, :], modp[:, l, 16:24]), reads=("modp",), writes=("P6",))
            T.add(DVE, o_stt(P6[:, l, 3, :], modp[:, l, 32:40], 1.0, n2w[:, l, :], ALU.add, ALU.mult),
                  reads=("modp", "n2w"), writes=("P6",))
            T.add(DVE, o_copy(P6[:, l, 4, :], modp[:, l, 24:32]), reads=("modp",), writes=("P6",))
            T.add(DVE, o_copy(P6[:, l, 5, :], modp[:, l, 40:48]), reads=("modp",), writes=("P6",))
        T.add(ACT, o_act(lbr[:], lbr[:], AF.Exp), reads=("lbr",), writes=("lbr",))
        T.add(DVE, o_tt(lbs[:], lbr[:, 0, :], lbr[:, 1, :], ALU.add), reads=("lbr",), writes=("lbs",))
        T.add(DVE, o_tt(lbs[:], lbs[:], lbr[:, 2, :], ALU.add), reads=("lbr", "lbs"), writes=("lbs",))
        T.add(DVE, o_tt(lbs[:], lbs[:], lbr[:, 3, :], ALU.add), reads=("lbr", "lbs"), writes=("lbs",))
        T.add(DVE, o_recip(lbs[:], lbs[:]), reads=("lbs",), writes=("lbs",))
        T.add(DVE, o_tt(lbr[:], lbr[:], bc(lbs[:], 1, [64, DEPTH, 4]), ALU.mult), reads=("lbr", "lbs"),
              writes=("lbr",))
        T.add(DVE, o_memset(lbm[:, 0, :], 0.0), writes=("lbm",))
        T.add(DVE, o_copy(lbm[:, 1, :], lbr[:, 1, :]), reads=("lbr",), writes=("lbm",))
        T.add(DVE, o_tt(lbm[:, 2, :], lbm[:, 1, :], lbr[:, 2, :], ALU.add), reads=("lbr", "lbm"), writes=("lbm",))
        T.add(DVE, o_tt(lbm[:, 3, :], lbm[:, 2, :], lbr[:, 3, :], ALU.add), reads=("lbr", "lbm"), writes=("lbm",))
        T.add(DVE, o_ts(lbm[:], lbm[:], 0.0, ALU.max), reads=("lbm",), writes=("lbm",))
        T.add(DVE, o_ts(lbm[:], lbm[:], -1.0, ALU.mult, 1.0, ALU.add), reads=("lbm",), writes=("lbm",))
        tap("P6", P6[:, 0:n_layers], "P6")
        tap("lbm", lbm[:], "lbm")
        T.barrier()
        arena.reset(mark0)

    for l in range(n_layers):
        src_d = xT_d if l == 0 else y_d
        phase_a(nc, arena, T, ps, PSR, l, src_d, y_d, dict(
            identb=identb, onesb=onesb, bonesb=bonesb, cmask=cmask, P6=P6, pscale=pscale, lbm=lbm, hnw=hnw,
            qnw=qnw, knw=knw, esink=esink, win_d=win_d, pwbd_d=pwbd_d, wop_d=wop_d, woh_d=woh_d, woa_d=woa_d,
            emask_d=emask_d, cinv_d=cinv_d, invw_d=invw_d), tap, load, stop_a_tiles)
        T.barrier()
        if do_b:
            phase_b(nc, arena, T, ps, PSR, l, y_d, dict(
                identb=identb, onesb=onesb, sel2=sel2, P6=P6, rw=rw, rb=rb, wg_d=wg_d, wu_d=wu_d, wd_d=wd_d),
                tap, load)
            T.barrier()

    cnt = T.emit(nc, st)
    st.close()
    return nc, cnt, (len(T.ops), arena.peak)


def interleave(gens):
    gens = [g for g in gens if g is not None]
    while gens:
        for g in list(gens):
            try:
                next(g)
            except StopIteration:
                gens.remove(g)


def drain(g):
    for _ in g:
        pass


def phase_a(nc, arena, T, ps, PSR, l, src_d, y_d, G, tap, load, stop_a_tiles, pipeline=True):
    identb, onesb, bonesb, cmask, P6 = G["identb"], G["onesb"], G["bonesb"], G["cmask"], G["P6"]
    pscale, lbm, hnw, qnw, knw, esink = G["pscale"], G["lbm"], G["hnw"], G["qnw"], G["knw"], G["esink"]
    mark = arena.mark()

    def sb(name, shape, dt=F32):
        return arena.alloc(list(shape), dt)

    NCH = TA // 64
    NBK = TA // 128
    NAV = 6
    win = sb("win", [128, 8, 2048], BF16)
    wop = sb("wop", [128, 2, D], BF16)
    woh = sb("woh", [64, 4, D], BF16)
    woa = sb("woa", [128, 4, D], BF16)
    pwbd = sb("pwbd", [128, 2, 128], BF16)
    emask = sb("emask", [128, 2, 8, 128], BF16)
    cinv = sb("cinv", [128, 2, TA])
    invw = sb("invw", [128, 2])
    xt = [sb("xt%d" % i, [128, 8, TA]) for i in range(2)]
    sq = sb("sq", [128, 8, TA], BF16)
    hT = sb("hT", [128, 8, TA], BF16)
    tmp = sb("tmp", [128, 8, TA])
    rstd = sb("rstd", [128, TA])
    abuf = sb("abuf", [128, 2, 16 + TA])
    s2 = sb("s2", [128, 2, 16 + TA])
    s4 = sb("s4", [128, 2, 16 + TA])
    s8 = sb("s8", [128, 16 + TA])
    s16 = sb("s16", [128, 16 + TA])
    pooled = sb("pooled", [128, 2, TA])
    pooledb = sb("pooledb", [128, 2, TA], BF16)
    ypT = sb("ypT", [128, 2, TA], BF16)
    off_hA = arena.mark()
    hA2 = [sb("hA%d" % i, [64, 4, TA]) for i in range(2)]
    qs2 = [sb("qs%d" % i, [64, 4, TA]) for i in range(2)]
    wohf = arena.alloc([64, 4, D], F32, at=off_hA)
    gs2 = [sb("gs%d" % i, [64, 4, TA]) for i in range(2)]
    hv2 = [sb("hv%d" % i, [64, TA // 64, 256], BF16) for i in range(2)]
    hK = sb("hK", [64, 4, TA])
    hB = sb("hB", [64, 4, TA])
    hC = sb("hC", [64, 4, TA])
    hE = sb("hE", [64, 4, TA])
    onesf = sb("onesf", [64, TA])
    qtl = sb("qtl", [64, 4, TA], BF16)
    ktl = sb("ktl", [64, 4, TA], BF16)
    ktok = sb("ktok", [64, 4, 64], BF16)
    scT = sb("scT", [64, 4, 64], BF16)
    Up = sb("Up", [64, 4, 64])
    Sst = sb("Sst", [64, 4, 64])
    Stl = sb("Stl", [64, 4, 64], BF16)
    eL = sb("eL", [64, 4, NCH])
    eLm = sb("eLm", [64, 4, NCH])
    eM = sb("eM", [64, 4, NCH])
    yhT = sb("yhT", [64, 4, TA], BF16)
    oT = hC
    rso = hE
    sqo = hB.bitcast(BF16)[:, :, 0:TA]
    aq2 = [sb("aq%d" % i, [128, 4, TA]) for i in range(2)]
    sqa2 = [sb("sqa%d" % i, [128, 4, TA], BF16) for i in range(2)]
    ak2 = [sb("ak%d" % i, [128, TA]) for i in range(2)]
    sqk2 = [sb("sqk%d" % i, [128, TA], BF16) for i in range(2)]
    rq = sb("rq", [128, 4, TA])
    aqn = sb("aqn", [128, 4, TA], BF16)
    rk = sb("rk", [128, TA])
    akn = sb("akn", [128, 4, 128], BF16)
    av = sb("av", [128, NAV, 2, 65], BF16)
    pex = [sb("pex%d" % i, [128, 512]) for i in range(2)]
    pT = sb("pT", [128, 2, 8, 128], BF16)
    den = sb("den", [128, 8])
    ytok = sb("ytok", [128, 8, 64], BF16)
    yaT = sb("yaT", [128, 4, TA], BF16)

    for q in (1, 0, 2, 3):
        load(win[:, :, q * 512:(q + 1) * 512], G["win_d"][l, :, :, q * 512:(q + 1) * 512], ("win", q), eng=POOL)
    HALL = (("hA", 0), ("hA", 1), ("qs", 0), ("qs", 1))
    T.add(SP, o_dma(wohf[:], G["woh_d"][l]), reads=(), writes=("wohf",) + HALL, chan="ld_wohf")
    load(wop[:], G["wop_d"][l], "wop", eng=POOL)
    load(woa[:], G["woa_d"][l], "woa", eng=POOL)
    load(pwbd[:], G["pwbd_d"][:, l], "pwbd", eng=POOL)
    load(emask[:], G["emask_d"], "emask", eng=POOL)
    load(cinv[:], G["cinv_d"][:, 0], "cinv")
    load(invw[:], G["invw_d"], "invw")
    T.add(DVE, o_tt(woh[:], wohf[:], bc(hnw[:, l, :], 2, [64, 4, D]), ALU.mult), reads=("wohf", "hnw"),
          writes=("woh",) + HALL)
    T.add(DVE, o_memset(abuf[:, :, 0:16], 0.0), writes=("abuf",))
    T.add(DVE, o_memset(Sst[:], 0.0), writes=("Sst",))
    T.add(DVE, o_memset(onesf[:], 1.0), writes=("onesf",))
    T.add(DVE, o_memset(av[:, :, :, 64:65], 1.0), writes=("av",))
    WIN = tuple(("win", q) for q in range(4))

    def winq(col0, ncol):
        return tuple(("win", q) for q in range(col0 // 512, (col0 + ncol - 1) // 512 + 1))

    A1 = P6[:, l, 0, :]
    B1 = P6[:, l, 1, :]
    g1 = P6[:, l, 2, :]
    xdv = src_d.rearrange("(c p) t -> p c t", p=128)
    ydv = y_d.rearrange("(c p) t -> p c t", p=128)
    ntiles = NTA if stop_a_tiles is None else stop_a_tiles

    def load_x(j):
        load(xt[j % 2][:], xdv[:, :, j * TA:(j + 1) * TA], ("xt", j % 2))

    def front(j):
        par = j % 2
        xs = xt[par]
        XR = ("xt", par)
        hA, qs, gs, hv = hA2[par], qs2[par], gs2[par], hv2[par]
        aq, sqa, ak, sqk = aq2[par], sqa2[par], ak2[par], sqk2[par]
        T.add(ACT, o_act(sq[:], xs[:], AF.Square), reads=(XR,), writes=("sq",))
        for c in range(8):
            T.add(PE, o_mm(ps[0][:, 0:TA], onesb[:], sq[:, c, :], start=(c == 0), stop=(c == 7)),
                  reads=("sq", "onesb"), writes=(PSR(0),))
        T.add(ACT, o_act(rstd[:], ps[0][:, 0:TA], AF.Ln, bias=EPS, scale=1.0 / D), reads=(PSR(0),), writes=("rstd",))
        T.add(ACT, o_act(rstd[:], rstd[:], AF.Exp, scale=-0.5), reads=("rstd",), writes=("rstd",))
        yield
        T.add(DVE, o_tt(tmp[:], xs[:], bc(rstd[:], 1, [128, 8, TA]), ALU.mult), reads=(XR, "rstd"), writes=("tmp",))
        for c in range(8):
            T.add(POOL, o_ts(hT[:, c, :], tmp[:, c, :], A1[:, c:c + 1], ALU.mult, B1[:, c:c + 1], ALU.add),
                  reads=("tmp", "P6"), writes=("hT",))
        yield
        if j == 0:
            tap("hT", hT[:], "hT")
        rr = [0]

        def bank():
            rr[0] = (rr[0] + 1) % 3
            return rr[0]

        def proj(b, col0, ncol, off, M=128):
            for c in range(8):
                T.add(PE, o_mm(ps[b][0:M, off:off + TA], win[:, c, col0:col0 + ncol], hT[:, c, :],
                               start=(c == 0), stop=(c == 7)), reads=winq(col0, ncol) + ("hT",), writes=(PSR(b),))

        def view2(b, M=128):
            return ps[b][0:M, 0:2 * TA].rearrange("p (c t) -> p c t", c=2)

        for hp in range(2):
            b = bank()
            proj(b, 512 + (2 * hp) * 64, 64, 0, M=64)
            proj(b, 512 + (2 * hp + 1) * 64, 64, TA, M=64)
            T.add(ACT, o_act(hA[:, 2 * hp:2 * hp + 2, :], view2(b, 64), AF.Exp), reads=(PSR(b),), writes=(("hA", par),))
            yield
        b = bank()
        proj(b, 0, 128, 0)
        proj(b, 128, 128, TA)
        T.add(ACT, o_act(abuf[:, :, 16:16 + TA], view2(b), AF.Copy), reads=(PSR(b),), writes=("abuf",))
        yield
        for qp in range(2):
            b = bank()
            proj(b, 1280 + (2 * qp) * 128, 128, 0)
            proj(b, 1280 + (2 * qp + 1) * 128, 128, TA)
            T.add(ACT, o_act(aq[:, 2 * qp:2 * qp + 2, :], view2(b), AF.Copy), reads=(PSR(b),), writes=(("aq", par),))
            T.add(ACT, o_act(sqa[:, 2 * qp:2 * qp + 2, :], view2(b), AF.Square), reads=(PSR(b),),
                  writes=(("sqa", par),))
            yield
        b = bank()
        proj(b, 1792, 128, 0)
        T.add(ACT, o_act(ak[:], ps[b][:, 0:TA], AF.Copy), reads=(PSR(b),), writes=(("ak", par),))
        T.add(ACT, o_act(sqk[:], ps[b][:, 0:TA], AF.Square), reads=(PSR(b),), writes=(("sqk", par),))
        yield
        for cp in range(NCH // 2):
            b = bank()
            for k2 in range(2):
                cc = cp * 2 + k2
                for c in range(8):
                    T.add(PE, o_mm(ps[b][0:64, k2 * 256:(k2 + 1) * 256], hT[:, c, cc * 64:(cc + 1) * 64],
                                   win[:, c, 768:1024], start=(c == 0), stop=(c == 7)),
                          reads=winq(768, 256) + ("hT",), writes=(PSR(b),))
            T.add(DVE, o_copy(hv[:, cp * 2:cp * 2 + 2, :], ps[b][0:64, :].rearrange("p (c t) -> p c t", c=2)),
                  reads=(PSR(b),), writes=(("hv", par),))
            yield
        b = bank()
        for bk in range(NBK):
            for c in range(8):
                T.add(PE, o_mm(ps[b][:, bk * 128:(bk + 1) * 128], hT[:, c, bk * 128:(bk + 1) * 128],
                               win[:, c, 1920:2048], start=(c == 0), stop=(c == 7)),
                      reads=winq(1920, 128) + ("hT",), writes=(PSR(b),))
        slot0 = (j * NBK) % NAV
        T.add(DVE, o_copy(av[:, slot0:slot0 + NBK, :, 0:64],
                          ps[b][:, 0:NBK * 128].rearrange("p (b k d) -> p b k d", b=NBK, k=2)),
              reads=(PSR(b),), writes=("av",))
        yield
        for hp in range(2):
            b = bank()
            proj(b, 256 + (2 * hp) * 64, 64, 0, M=64)
            proj(b, 256 + (2 * hp + 1) * 64, 64, TA, M=64)
            T.add(ACT, o_act(qs[:, 2 * hp:2 * hp + 2, :], view2(b, 64), AF.Silu), reads=(PSR(b),), writes=(("qs", par),))
            yield
        for hp in range(2):
            b = bank()
            proj(b, 1024 + (2 * hp) * 64, 64, 0, M=64)
            proj(b, 1024 + (2 * hp + 1) * 64, 64, TA, M=64)
            T.add(ACT, o_act(gs[:, 2 * hp:2 * hp + 2, :], view2(b, 64), AF.Silu), reads=(PSR(b),), writes=(("gs", par),))
            yield

    def pool_pre(j):
        W = 16 + TA
        T.add(POOL, o_tt(s2[:, :, 1:W], abuf[:, :, 1:W], abuf[:, :, 0:W - 1], ALU.add), reads=("abuf",), writes=("s2",))
        T.add(POOL, o_tt(s4[:, :, 3:W], s2[:, :, 3:W], s2[:, :, 1:W - 2], ALU.add), reads=("s2",), writes=("s4",))
        T.add(POOL, o_tt(s8[:, 7:W], s4[:, 1, 7:W], s4[:, 1, 3:W - 4], ALU.add), reads=("s4",), writes=("s8",))
        T.add(POOL, o_tt(s16[:, 15:W], s8[:, 15:W], s8[:, 7:W - 8], ALU.add), reads=("s8",), writes=("s16",))
        srcs = [(s2[0:64, 0, 16:W], slice(0, 64), 0, "s2"), (s4[64:128, 0, 16:W], slice(64, 128), 0, "s4"),
                (s8[0:64, 16:W], slice(0, 64), 1, "s8"), (s16[64:128, 16:W], slice(64, 128), 1, "s16")]
        for sap, rows, cp, rn in srcs:
            if j == 0:
                T.add(POOL, o_tt(pooled[rows, cp, :], sap, cinv[rows, cp, :], ALU.mult), reads=(rn, "cinv"),
                      writes=("pooled",))
            else:
                T.add(POOL, o_ts(pooled[rows, cp, :], sap, invw[rows, cp:cp + 1], ALU.mult, 0.0, ALU.add),
                      reads=(rn, "invw"), writes=("pooled",))
        T.add(POOL, o_tt(pooledb[:], pooled[:], abuf[:, :, 16:W], ALU.subtract), reads=("pooled", "abuf"),
              writes=("pooledb",))
        T.add(POOL, o_copy(abuf[:, :, 0:16], abuf[:, :, TA:TA + 16]), reads=("abuf",), writes=("abuf",))
        if j == 0:
            tap("pooled", pooledb[:], "pooledb")

    def pool_post(j, b):
        for cp in range(2):
            T.add(PE, o_mm(ps[b][:, cp * TA:(cp + 1) * TA], pwbd[:, cp, :], pooledb[:, cp, :]),
                  reads=("pwbd", "pooledb"), writes=(PSR(b),))
        for cp in range(2):
            T.add(ACT, o_act(ypT[:, cp, :], ps[b][:, cp * TA:(cp + 1) * TA], AF.Copy, scale=pscale[:, l, cp:cp + 1]),
                  reads=(PSR(b), "pscale"), writes=("ypT",))

    def hgrn(j):
        par = j % 2
        hA, qs, gs, hv = hA2[par], qs2[par], gs2[par], hv2[par]
        HA = ("hA", par)
        BA, BB = 3, 4
        T.add(ACT, o_act(hA[:], hA[:], AF.Ln, bias=1.0), reads=(HA,), writes=(HA,))
        T.add(ACT, o_act(hA[:], hA[:], AF.Exp, scale=-1.0), reads=(HA,), writes=(HA,))
        yield
        T.add(DVE, o_tt(hK[:], hA[:], bc(lbm[:, l, :], 2, [64, 4, TA]), ALU.mult), reads=(HA, "lbm"), writes=("hK",))
        T.add(DVE, o_ts(hA[:], hK[:], MAXK, ALU.min), reads=("hK",), writes=(HA,))
        T.add(ACT, o_act(hB[:], hA[:], AF.Ln, bias=1.0, scale=-1.0), reads=(HA,), writes=("hB",))
        yield
        for h in range(4):
            T.add(DVE, o_scan(hC[:, h, :], onesf[:], hB[:, h, :], 0.0, ALU.mult, ALU.add), reads=("hB", "onesf"),
                  writes=("hC",))
        yield
        hC4 = hC[:].rearrange("p h (c t) -> p h c t", t=64)
        hA4 = hA[:].rearrange("p h (c t) -> p h c t", t=64)
        hB4 = hB[:].rearrange("p h (c t) -> p h c t", t=64)
        T.add(DVE, o_copy(hA4[:, :, 0, :], hC4[:, :, 0, :]), reads=("hC",), writes=(HA,))
        T.add(DVE, o_tt(hA4[:, :, 1:NCH, :], hC4[:, :, 1:NCH, :],
                        bc(hC4[:, :, 0:NCH - 1, 63], 3, [64, 4, NCH - 1, 64]), ALU.subtract), reads=("hC",), writes=(HA,))
        yield
        T.add(ACT, o_act(eL[:], hA4[:, :, :, 63], AF.Exp), reads=(HA,), writes=("eL",))
        T.add(ACT, o_act(eM[:], hA4[:, :, :, 31], AF.Exp), reads=(HA,), writes=("eM",))
        T.add(DVE, o_tt(eLm[:], hA4[:, :, :, 63], hA4[:, :, :, 31], ALU.subtract), reads=(HA,), writes=("eLm",))
        T.add(ACT, o_act(eLm[:], eLm[:], AF.Exp), reads=("eLm",), writes=("eLm",))
        T.add(DVE, o_tt(hB4, hA4, bc(hA4[:, :, :, 31], 3, [64, 4, NCH, 64]), ALU.subtract), reads=(HA,), writes=("hB",))
        yield
        T.add(ACT, o_act(hE[:], hB[:], AF.Exp), reads=("hB",), writes=("hE",))
        T.add(ACT, o_act(hC[:], hB[:], AF.Exp, scale=-1.0), reads=("hB",), writes=("hC",))
        yield
        T.add(DVE, o_tt(qtl[:], qs[:], hE[:], ALU.mult), reads=(("qs", par), "hE"), writes=("qtl",))
        T.add(DVE, o_tt(ktl[:], hK[:], hC[:], ALU.mult), reads=("hK", "hC"), writes=("ktl",))
        yield
        for cc in range(NCH):
            cs = slice(cc * 64, (cc + 1) * 64)
            psab = ps[BA][0:64, 0:128].bitcast(BF16)
            for h in range(4):
                T.add(PE, o_tr(psab[:, h * 64:(h + 1) * 64], ktl[:, h, cs], identb[0:64, 0:64]),
                      reads=("ktl", "identb"), writes=(PSR(BA),))
            for h in range(4):
                T.add(PE, o_mm(ps[BB][0:64, h * 64:(h + 1) * 64], ktl[:, h, cs], qtl[:, h, cs]),
                      reads=("ktl", "qtl"), writes=(PSR(BB),))
            T.add(ACT, o_act(ktok[:], psab.rearrange("p (h k) -> p h k", h=4), AF.Copy), reads=(PSR(BA),),
                  writes=("ktok",))
            T.add(DVE, o_tt(scT[:], ps[BB][0:64, 0:256].rearrange("p (h t) -> p h t", h=4),
                            bc(cmask[:], 1, [64, 4, 64]), ALU.mult), reads=(PSR(BB), "cmask"), writes=("scT",))
            T.add(DVE, o_tt(Stl[:], Sst[:], bc(eM[:, :, cc], 2, [64, 4, 64]), ALU.mult), reads=("Sst", "eM"),
                  writes=("Stl",))
            yield
            for h in range(4):
                T.add(PE, o_mm(ps[BA][0:64, h * 64:(h + 1) * 64], ktok[:, h, :], hv[:, cc, h * 64:(h + 1) * 64]),
                      reads=("ktok", ("hv", par)), writes=(PSR(BA),))
            for h in range(4):
                T.add(PE, o_mm(ps[BB][0:64, h * 64:(h + 1) * 64], hv[:, cc, h * 64:(h + 1) * 64], scT[:, h, :],
                               start=True, stop=False), reads=(("hv", par), "scT"), writes=(PSR(BB),))
                T.add(PE, o_mm(ps[BB][0:64, h * 64:(h + 1) * 64], Stl[:, h, :], qtl[:, h, cs],
                               start=False, stop=True), reads=("Stl", "qtl"), writes=(PSR(BB),))
            T.add(DVE, o_tt(Up[:], ps[BA][0:64, 0:256].rearrange("p (h t) -> p h t", h=4),
                            bc(eLm[:, :, cc], 2, [64, 4, 64]), ALU.mult), reads=(PSR(BA), "eLm"), writes=("Up",))
            T.add(DVE, o_tt(Sst[:], Sst[:], bc(eL[:, :, cc], 2, [64, 4, 64]), ALU.mult), reads=("Sst", "eL"),
                  writes=("Sst",))
            T.add(DVE, o_tt(Sst[:], Sst[:], Up[:], ALU.add), reads=("Sst", "Up"), writes=("Sst",))
            src = ps[BB][0:64, 0:256].rearrange("p (h t) -> p h t", h=4)
            T.add(ACT, o_act(oT[:, :, cs], src, AF.Copy), reads=(PSR(BB),), writes=("hC",))
            T.add(ACT, o_act(sqo[:, :, cs], src, AF.Square), reads=(PSR(BB),), writes=("hB",))
            yield
        for hp in range(2):
            b = BA if hp == 0 else BB
            for k2 in range(2):
                T.add(PE, o_mm(ps[b][0:64, k2 * TA:(k2 + 1) * TA], onesb[0:64, 0:64], sqo[:, 2 * hp + k2, :]),
                      reads=("hB", "onesb"), writes=(PSR(b),))
            T.add(ACT, o_act(rso[:, 2 * hp:2 * hp + 2, :], ps[b][0:64, 0:2 * TA].rearrange("p (c t) -> p c t", c=2),
                             AF.Ln, bias=EPS, scale=1.0 / 64), reads=(PSR(b),), writes=("hE",))
        yield
        T.add(ACT, o_act(rso[:], rso[:], AF.Exp, scale=-0.5), reads=("hE",), writes=("hE",))
        T.add(DVE, o_tt(oT[:], oT[:], rso[:], ALU.mult), reads=("hC", "hE"), writes=("hC",))
        T.add(DVE, o_tt(yhT[:], oT[:], gs[:], ALU.mult), reads=("hC", ("gs", par)), writes=("yhT",))
        if j == 0:
            tap("yhT", yhT[:], "yhT")
        yield

    def attn(j):
        par = j % 2
        aq, sqa, ak, sqk = aq2[par], sqa2[par], ak2[par], sqk2[par]
        for qp in range(2):
            b = 5 + qp
            for k2 in range(2):
                T.add(PE, o_mm(ps[b][:, k2 * TA:(k2 + 1) * TA], bonesb[:], sqa[:, 2 * qp + k2, :]),
                      reads=(("sqa", par), "bonesb"), writes=(PSR(b),))
            T.add(ACT, o_act(rq[:, 2 * qp:2 * qp + 2, :], ps[b][:, 0:2 * TA].rearrange("p (c t) -> p c t", c=2),
                             AF.Ln, bias=EPS, scale=1.0 / 64), reads=(PSR(b),), writes=("rq",))
        T.add(PE, o_mm(ps[7][:, 0:TA], bonesb[:], sqk[:]), reads=(("sqk", par), "bonesb"), writes=(PSR(7),))
        T.add(ACT, o_act(rk[:], ps[7][:, 0:TA], AF.Ln, bias=EPS, scale=1.0 / 64), reads=(PSR(7),), writes=("rk",))
        yield
        T.add(ACT, o_act(rq[:], rq[:], AF.Exp, scale=-0.5), reads=("rq",), writes=("rq",))
        T.add(ACT, o_act(rk[:], rk[:], AF.Exp, scale=-0.5), reads=("rk",), writes=("rk",))
        yield
        aslot0 = (j * NBK) % 4
        T.add(DVE, o_stt(aqn[:].rearrange("p c t -> p (c t)"), aq[:].rearrange("p c t -> p (c t)"),
                         qnw[:, l:l + 1], rq[:].rearrange("p c t -> p (c t)"), ALU.mult, ALU.mult),
              reads=(("aq", par), "rq", "qnw"), writes=("aqn",))
        T.add(DVE, o_stt(akn[:, aslot0:aslot0 + NBK, :].rearrange("p b t -> p (b t)"), ak[:], knw[:, l:l + 1], rk[:],
                         ALU.mult, ALU.mult), reads=(("ak", par), "rk", "knw"), writes=("akn",))
        yield
        for bk in range(NBK):
            gblk = j * NBK + bk
            rels = [(1, gblk)] if gblk == 0 else [(0, gblk - 1), (1, gblk)]
            qsl = slice(bk * 128, (bk + 1) * 128)
            ui = 0
            for rel, kb in rels:
                for hg in range(2):
                    b = 5 + (ui % 2)
                    px = pex[ui % 2]
                    PXR = ("pex", ui % 2)
                    ui += 1
                    for p in range(4):
                        T.add(PE, o_mm(ps[b][:, p * 128:(p + 1) * 128], akn[hg * 64:(hg + 1) * 64, kb % 4, :],
                                       aqn[hg * 64:(hg + 1) * 64, p, qsl]), reads=("akn", "aqn"), writes=(PSR(b),))
                    T.add(ACT, o_act(px[:], ps[b][:, :], AF.Exp, scale=0.125), reads=(PSR(b),), writes=(PXR,))
                    T.add(DVE, o_tt(pT[:, rel, hg * 4:hg * 4 + 4, :].rearrange("p h q -> p (h q)"), px[:],
                                    emask[:, rel, hg * 4:hg * 4 + 4, :].rearrange("p h q -> p (h q)"), ALU.mult),
                          reads=(PXR, "emask"), writes=("pT",))
                    yield
            for hb in range(2):
                b = 7 if hb == 0 else 5
                for hh in range(4):
                    h = hb * 4 + hh
                    for ri, (rel, kb) in enumerate(rels):
                        T.add(PE, o_mm(ps[b][:, hh * 128:hh * 128 + 65], pT[:, rel, h, :], av[:, kb % NAV, hb, :],
                                       start=(ri == 0), stop=(ri == len(rels) - 1)), reads=("pT", "av"),
                              writes=(PSR(b),))
                pv = ps[b][:, :].rearrange("p (h d) -> p h d", h=4)
                T.add(DVE, o_tt(den[:, hb * 4:hb * 4 + 4], pv[:, :, 64], esink[:, l, hb * 4:hb * 4 + 4], ALU.add),
                      reads=(PSR(b), "esink"), writes=("den",))
                T.add(DVE, o_recip(den[:, hb * 4:hb * 4 + 4], den[:, hb * 4:hb * 4 + 4]), reads=("den",), writes=("den",))
                T.add(DVE, o_tt(ytok[:, hb * 4:hb * 4 + 4, :], pv[:, :, 0:64],
                                bc(den[:, hb * 4:hb * 4 + 4], 2, [128, 4, 64]), ALU.mult),
                      reads=(PSR(b), "den"), writes=("ytok",))
                yield
            ps6b = ps[6][:, 0:256].bitcast(BF16)
            ytf = ytok[:].rearrange("p h d -> p (h d)")
            for kc in range(4):
                T.add(PE, o_tr(ps6b[:, kc * 128:(kc + 1) * 128], ytf[:, kc * 128:(kc + 1) * 128], identb[:]),
                      reads=("ytok", "identb"), writes=(PSR(6),))
            T.add(ACT, o_act(yaT[:, :, qsl], ps6b.rearrange("p (c t) -> p c t", c=4), AF.Copy), reads=(PSR(6),),
                  writes=("yaT",))
            yield
        if j == 0:
            tap("yaT", yaT[:], "yaT")

    def wout(j):
        par = j % 2
        xs = xt[par]
        XR = ("xt", par)
        pool_post(j, 2)
        if j == 0:
            tap("ypT", ypT[:], "ypT")
        for dc in range(8):
            b = 3 + (dc // 2) % 4
            off = (dc % 2) * TA
            dsl = slice(dc * 128, (dc + 1) * 128)
            out = ps[b][:, off:off + TA]
            for kc in range(2):
                T.add(PE, o_mm(out, wop[:, kc, dsl], ypT[:, kc, :], start=(kc == 0), stop=False),
                      reads=("wop", "ypT"), writes=(PSR(b),))
            for h in range(4):
                T.add(PE, o_mm(out, woh[:, h, dsl], yhT[:, h, :], start=False, stop=False),
                      reads=("woh", "yhT"), writes=(PSR(b),))
            for kc in range(4):
                T.add(PE, o_mm(out, woa[:, kc, dsl], yaT[:, kc, :], start=False, stop=(kc == 3)),
                      reads=("woa", "yaT"), writes=(PSR(b),))
            T.add(DVE, o_stt(tmp[:, dc, :], out, g1[:, dc:dc + 1], xs[:, dc, :], ALU.mult, ALU.add),
                  reads=(PSR(b), XR, "P6"), writes=("tmp",))
        T.add(SP, o_dma(ydv[:, :, j * TA:(j + 1) * TA], tmp[:]), reads=("tmp",), writes=(("y", j),), chan="yst")

    def limited(g, n):
        for _ in range(n):
            try:
                next(g)
            except StopIteration:
                return
            yield

    def delayed(g, k):
        for _ in range(k):
            yield
        yield from g

    load_x(0)
    if ntiles > 1:
        load_x(1)
    drain(front(0))
    for j in range(ntiles):
        pool_pre(j)
        gf = front(j + 1) if j + 1 < ntiles else None
        interleave([hgrn(j), attn(j), delayed(limited(gf, 2), 6) if gf is not None else None])
        if gf is not None:
            drain(gf)
        wout(j)
        if j + 2 < ntiles:
            load_x(j + 2)
    arena.reset(mark)


def phase_b(nc, arena, T, ps, PSR, l, y_d, G, tap, load):
    identb, onesb, sel2, P6, rw, rb = G["identb"], G["onesb"], G["sel2"], G["P6"], G["rw"], G["rb"]
    mark = arena.mark()

    def sb(name, shape, dt=F32):
        return arena.alloc(list(shape), dt)

    yacc = sb("yacc", [128, 8, TB])
    h2T = sb("h2T", [128, 8, TB], BF16)
    Wg = [sb("Wg%d" % i, [128, 8, 512], BF16) for i in range(2)]
    Wu = [sb("Wu%d" % i, [128, 8, 512], BF16) for i in range(2)]
    Wd = [sb("Wd%d" % i, [128, 4, D], BF16) for i in range(2)]
    Hb = [sb("Hb%d" % i, [128, 4, SUB], BF16) for i in range(2)]
    sqb = sb("sqb", [128, 8, SUB], BF16)
    tmpb = [sb("tmpb%d" % i, [128, SUB]) for i in range(2)]
    sg = [sb("sg%d" % i, [128, SUB]) for i in range(2)]
    t1 = [sb("t1%d" % i, [128, SUB]) for i in range(2)]
    gbc = [sb("gbc%d" % i, [128, SUB]) for i in range(2)]
    gT2 = sb("gT2", [48, TB], BF16)
    rstdb = sb("rstdb", [128, SUB])
    Wp = sb("Wp", [128, 8, NE])
    B2bc = sb("B2bc", [128, 8, 128])
    bpr = sb("bpr", [128, NE])
    NB = SUB // 128
    rtok = sb("rtok", [128, NB])
    lg = sb("lg", [128, NB, NE])
    sc = sb("sc", [128, NB, NE])
    se = sb("se", [128, NB, NE])
    se2 = sb("se2", [128, NB, NE])
    ch = sb("ch", [128, NB, NE])
    r1 = sb("r1", [128, NB])
    m1 = sb("m1", [128, NB, 4])
    m2 = sb("m2", [128, NB, 4])
    gsc = sb("gsc", [128, NB, 4])
    gts = sb("gts", [128, NB, NE])
    g3 = sb("g3", [128, NB, 48], BF16)

    A2 = P6[:, l, 3, :]
    B2 = P6[:, l, 4, :]
    g2 = P6[:, l, 5, :]
    RB = 7
    T.add(DVE, o_tt(Wp[:], rw[:], bc(A2, 2, [128, 8, NE]), ALU.mult), reads=("rw", "P6"), writes=("Wp",))
    T.add(DVE, o_copy(B2bc[:], bc(B2, 2, [128, 8, 128])), reads=("P6",), writes=("B2bc",))
    for c in range(8):
        T.add(PE, o_mm(ps[RB][:, 0:NE], B2bc[:, c, :], rw[:, c, :], start=(c == 0), stop=(c == 7)),
              reads=("B2bc", "rw"), writes=(PSR(RB),))
    T.add(DVE, o_copy(bpr[:], ps[RB][:, 0:NE]), reads=(PSR(RB),), writes=("bpr",))
    T.add(DVE, o_memset(g3[:], 0.0), writes=("g3",))

    ydv = y_d.rearrange("(c p) t -> p c t", p=128)

    def load_expert(e):
        k = e % 2
        load(Wg[k][:], G["wg_d"][l, e], ("Wg", k), eng=POOL)
        load(Wu[k][:], G["wu_d"][l, e], ("Wu", k), eng=POOL)
        load(Wd[k][:], G["wd_d"][l, e], ("Wd", k), eng=POOL)

    def norm_route(s_):
        ss = slice(s_ * SUB, (s_ + 1) * SUB)
        YR = ("yacc", s_)
        T.add(ACT, o_act(sqb[:], yacc[:, :, ss], AF.Square), reads=(YR,), writes=("sqb",))
        for c in range(8):
            T.add(PE, o_mm(ps[RB][:, :], onesb[:], sqb[:, c, :], start=(c == 0), stop=(c == 7)),
                  reads=("sqb", "onesb"), writes=(PSR(RB),))
        T.add(ACT, o_act(rstdb[:], ps[RB][:, :], AF.Ln, bias=EPS, scale=1.0 / D), reads=(PSR(RB),), writes=("rstdb",))
        T.add(ACT, o_act(rstdb[:], rstdb[:], AF.Exp, scale=-0.5), reads=("rstdb",), writes=("rstdb",))
        yield
        for c in range(8):
            tb = tmpb[c % 2]
            TR = ("tmpb", c % 2)
            T.add(DVE, o_tt(tb[:], yacc[:, c, ss], rstdb[:], ALU.mult), reads=(YR, "rstdb"), writes=(TR,))
            T.add(POOL, o_ts(h2T[:, c, ss], tb[:], A2[:, c:c + 1], ALU.mult, B2[:, c:c + 1], ALU.add),
                  reads=(TR, "P6"), writes=(("h2T", s_),))
            if c % 2 == 1:
                yield
        for bk in range(NB):
            tsl = slice(s_ * SUB + bk * 128, s_ * SUB + (bk + 1) * 128)
            for c in range(8):
                T.add(PE, o_mm(ps[RB][:, bk * NE:(bk + 1) * NE], yacc[:, c, tsl], Wp[:, c, :],
                               start=(c == 0), stop=(c == 7)), reads=(YR, "Wp"), writes=(PSR(RB),))
            for c in range(8):
                T.add(PE, o_mm(ps[RB][:, 64 + bk:65 + bk], sqb[:, c, bk * 128:(bk + 1) * 128], onesb[:, 0:1],
                               start=(c == 0), stop=(c == 7)), reads=("sqb", "onesb"), writes=(PSR(RB),))
            yield
        T.add(ACT, o_act(rtok[:], ps[RB][:, 64:64 + NB], AF.Ln, bias=EPS, scale=1.0 / D), reads=(PSR(RB),),
              writes=("rtok",))
        T.add(ACT, o_act(rtok[:], rtok[:], AF.Exp, scale=-0.5), reads=("rtok",), writes=("rtok",))
        L3 = ps[RB][:, 0:NB * NE].rearrange("p (b e) -> p b e", e=NE)
        T.add(DVE, o_tt(lg[:], L3, bc(rtok[:], 2, [128, NB, NE]), ALU.mult), reads=(PSR(RB), "rtok"), writes=("lg",))
        T.add(DVE, o_tt(lg[:], lg[:], bc(bpr[:], 1, [128, NB, NE]), ALU.add), reads=("lg", "bpr"), writes=("lg",))
        T.add(DVE, o_red(r1[:], lg[:], ALU.max), reads=("lg",), writes=("r1",))
        T.add(DVE, o_tt(lg[:], lg[:], bc(r1[:], 2, [128, NB, NE]), ALU.subtract), reads=("lg", "r1"), writes=("lg",))
        yield
        T.add(ACT, o_act(sc[:], lg[:], AF.Exp), reads=("lg",), writes=("sc",))
        T.add(DVE, o_red(r1[:], sc[:], ALU.add), reads=("sc",), writes=("r1",))
        T.add(DVE, o_recip(r1[:], r1[:]), reads=("r1",), writes=("r1",))
        T.add(DVE, o_tt(sc[:], sc[:], bc(r1[:], 2, [128, NB, NE]), ALU.mult), reads=("sc", "r1"), writes=("sc",))
        T.add(DVE, o_tt(se[:], sc[:], bc(rb[:], 1, [128, NB, NE]), ALU.add), reads=("sc", "rb"), writes=("se",))
        yield
        se4 = se[:].rearrange("p b (g k) -> p b g k", k=4)
        se24 = se2[:].rearrange("p b (g k) -> p b g k", k=4)
        ch4 = ch[:].rearrange("p b (g k) -> p b g k", k=4)
        T.add(DVE, o_red(m1[:], se4, ALU.max), reads=("se",), writes=("m1",))
        T.add(DVE, o_tt(ch4, se4, bc(m1[:], 3, [128, NB, 4, 4]), ALU.is_equal), reads=("se", "m1"), writes=("ch",))
        T.add(DVE, o_stt(se2[:].rearrange("p b e -> p (b e)"), ch[:].rearrange("p b e -> p (b e)"), -1e9,
                         se[:].rearrange("p b e -> p (b e)"), ALU.mult, ALU.add), reads=("ch", "se"), writes=("se2",))
        T.add(DVE, o_red(m2[:], se24, ALU.max), reads=("se2",), writes=("m2",))
        yield
        T.add(DVE, o_tt(gsc[:], m1[:], m2[:], ALU.add), reads=("m1", "m2"), writes=("gsc",))
        T.add(DVE, o_red(r1[:], gsc[:], ALU.max), reads=("gsc",), writes=("r1",))
        T.add(DVE, o_tt(gsc[:], gsc[:], bc(r1[:], 2, [128, NB, 4]), ALU.is_equal), reads=("gsc", "r1"), writes=("gsc",))
        T.add(DVE, o_tt(ch4, se4, bc(m2[:], 3, [128, NB, 4, 4]), ALU.is_ge), reads=("se", "m2"), writes=("ch",))
        T.add(DVE, o_tt(ch4, ch4, bc(gsc[:], 3, [128, NB, 4, 4]), ALU.mult), reads=("ch", "gsc"), writes=("ch",))
        yield
        T.add(DVE, o_tt(gts[:], sc[:], ch[:], ALU.mult), reads=("sc", "ch"), writes=("gts",))
        T.add(DVE, o_red(r1[:], gts[:], ALU.add), reads=("gts",), writes=("r1",))
        T.add(DVE, o_recip(r1[:], r1[:]), reads=("r1",), writes=("r1",))
        T.add(DVE, o_tt(gts[:], gts[:], bc(r1[:], 2, [128, NB, NE]), ALU.mult), reads=("gts", "r1"), writes=("gts",))
        T.add(DVE, o_copy(g3[:, :, 0:NE], gts[:]), reads=("gts",), writes=("g3",))
        T.add(DVE, o_tt(g3[:, :, 32:48], gts[:], g3[:, :, 0:NE], ALU.subtract), reads=("gts", "g3"), writes=("g3",))
        yield
        psb = ps[RB][0:48, 0:256].bitcast(BF16)
        for bk in range(NB):
            T.add(PE, o_tr(psb[:, bk * 128:(bk + 1) * 128], g3[:, bk, :], identb[:]), reads=("g3", "identb"),
                  writes=(PSR(RB),))
        T.add(ACT, o_act(gT2[:, ss], psb, AF.Copy), reads=(PSR(RB),), writes=(("gT2", s_),))
        yield

    items = [(e, s_) for e in range(NE) for s_ in range(NSUB)]

    def emit_D(n):
        e, s_ = items[n]
        k = e % 2
        hb = Hb[n % 2]
        HR = ("Hb", n % 2)
        ss = slice(s_ * SUB, (s_ + 1) * SUB)
        for dc in range(8):
            b = 5 + dc % 2
            for jc in range(4):
                T.add(PE, o_mm(ps[b][:, :], Wd[k][:, jc, dc * 128:(dc + 1) * 128], hb[:, jc, :],
                               start=(jc == 0), stop=(jc == 3)), reads=(("Wd", k), HR), writes=(PSR(b),))
            T.add(DVE, o_stt(yacc[:, dc, ss], ps[b][:, :], g2[:, dc:dc + 1], yacc[:, dc, ss], ALU.mult, ALU.add),
                  reads=(PSR(b), ("yacc", s_), "P6"), writes=(("yacc", s_),))
            if dc % 2 == 1:
                yield

    def emit_GU(n):
        e, s_ = items[n]
        k = e % 2
        hb = Hb[n % 2]
        HR = ("Hb", n % 2)
        ss = slice(s_ * SUB, (s_ + 1) * SUB)
        gb_ = gbc[n % 2]
        GR = ("gbc", n % 2)
        T.add(PE, o_mm(ps[4][:, :], sel2[:, e, :], gT2[:, ss]), reads=("sel2", ("gT2", s_)), writes=(PSR(4),))
        T.add(ACT, o_act(gb_[:], ps[4][:, :], AF.Copy), reads=(PSR(4),), writes=(GR,))
        for jc in range(4):
            bg = jc % 2
            bu = 2 + jc % 2
            for c in range(8):
                T.add(PE, o_mm(ps[bg][:, :], Wg[k][:, c, jc * 128:(jc + 1) * 128], h2T[:, c, ss],
                               start=(c == 0), stop=(c == 7)), reads=(("Wg", k), ("h2T", s_)), writes=(PSR(bg),))
            for c in range(8):
                T.add(PE, o_mm(ps[bu][:, :], Wu[k][:, c, jc * 128:(jc + 1) * 128], h2T[:, c, ss],
                               start=(c == 0), stop=(c == 7)), reads=(("Wu", k), ("h2T", s_)), writes=(PSR(bu),))
            T.add(ACT, o_act(sg[jc % 2][:], ps[bg][:, :], AF.Silu), reads=(PSR(bg),), writes=(("sg", jc % 2),))
            T.add(DVE, o_tt(t1[jc % 2][:], ps[bu][:, :], sg[jc % 2][:], ALU.mult),
                  reads=(PSR(bu), ("sg", jc % 2)), writes=(("t1", jc % 2),))
            T.add(DVE, o_tt(hb[:, jc, :], t1[jc % 2][:], gb_[:], ALU.mult), reads=(("t1", jc % 2), GR), writes=(HR,))
            yield

    def item(n):
        yield from emit_GU(n)
        if n >= 1:
            yield from emit_D(n - 1)

    def store_y(J, s_):
        T0 = J * TB
        T.add(SP, o_dma(ydv[:, :, T0 + s_ * SUB:T0 + (s_ + 1) * SUB], yacc[:, :, s_ * SUB:(s_ + 1) * SUB]),
              reads=(("yacc", s_),), writes=(("yB", J, s_),), chan="ystb%d" % s_)

    def load_y(J, s_):
        T0 = J * TB
        load(yacc[:, :, s_ * SUB:(s_ + 1) * SUB], ydv[:, :, T0 + s_ * SUB:T0 + (s_ + 1) * SUB], ("yacc", s_))

    for s_ in range(NSUB):
        load_y(0, s_)
    load_expert(0)
    drain(norm_route(0))
    for J in range(NTB):
        last = (J + 1 == NTB)
        for n, (e, s_) in enumerate(items):
            gens = [item(n)]
            if e == 0 and s_ + 1 < NSUB:
                gens.append(norm_route(s_ + 1))
            if e == NE - 1 and s_ == 2 and not last:
                gens.append(norm_route(0))
            interleave(gens)
            if s_ == 0 and e + 1 < NE:
                load_expert(e + 1)
            if e == NE - 1:
                if s_ >= 1:
                    store_y(J, s_ - 1)
                    if not last:
                        load_y(J + 1, s_ - 1)
                if s_ == 0 and not last:
                    load_expert(0)
        drain(emit_D(len(items) - 1))
        store_y(J, NSUB - 1)
        if not last:
            load_y(J + 1, NSUB - 1)
    arena.reset(mark)


def _consts():
    ident = np.eye(128, dtype=np.float32)
    slopes = np.exp2(-8.0 * np.arange(1, 9, dtype=np.float32) / 8).astype(np.float32)
    k = np.arange(128)[:, None]
    q = np.arange(128)[None, :]
    emask = np.zeros((128, 2, 8, 128), np.float32)
    for h in range(8):
        d_prev = (q + 128 - k).astype(np.float32)
        emask[:, 0, h, :] = np.where(k > q, np.exp(-slopes[h] * d_prev), 0.0)
        d_own = (q - k).astype(np.float32)
        emask[:, 1, h, :] = np.where(q >= k, np.exp(-slopes[h] * d_own), 0.0)
    s = np.arange(64)[:, None]
    t = np.arange(64)[None, :]
    cmask = (s <= t).astype(np.float32)
    sel = np.zeros((48, NE, 128), np.float32)
    for e in range(NE):
        sel[e, e, :] = 1.0
        sel[32 + e, e, :] = 1.0
    cinv = np.zeros((128, 2, 2, TA), np.float32)
    wins = (2, 4, 8, 16)
    pos = np.arange(TA, dtype=np.float32)
    for cp in range(2):
        for half in range(2):
            w = wins[2 * cp + half]
            rows = slice(half * 64, (half + 1) * 64)
            cinv[rows, 0, cp, :] = 1.0 / np.minimum(pos + 1.0, float(w))
            cinv[rows, 1, cp, :] = 1.0 / float(w)
    bones = np.zeros((128, 128), np.float32)
    bones[0:64, 0:64] = 1.0
    bones[64:128, 64:128] = 1.0
    invw = np.ascontiguousarray(cinv[:, 1, :, 0])
    return dict(ident=ident, emask=emask, cmask=cmask, sel=sel, cinv=cinv, bones=bones, invw=invw)


def prep_shared(inp):
    f = lambda a: np.ascontiguousarray(np.asarray(a, dtype=np.float32))
    sh = {}
    sh["ada_w"] = f(inp["ada_w"])
    sh["ada_b"] = f(np.asarray(inp["ada_b"]).reshape(DEPTH, 48, 128).transpose(2, 0, 1))
    sh["n1w"] = f(np.asarray(inp["norm1_w"]).reshape(DEPTH, 8, 128).transpose(2, 0, 1))
    sh["n2w"] = f(np.asarray(inp["norm2_w"]).reshape(DEPTH, 8, 128).transpose(2, 0, 1))
    w_in = np.asarray(inp["w_in"], dtype=np.float32)
    cols = np.arange(2048)
    aq = 1280 + np.array([[(p + 4 * half) * 64 + d for half in range(2) for d in range(64)] for p in range(4)]).reshape(-1)
    cols[1280:1792] = aq
    w_in = w_in[:, :, cols]
    sh["w_in"] = f(w_in.reshape(DEPTH, 8, 128, 2048).transpose(0, 2, 1, 3))
    pw = np.asarray(inp["pool_w"], dtype=np.float32)
    pwbd = np.zeros((128, DEPTH, 2, 128), np.float32)
    for l in range(DEPTH):
        for cp in range(2):
            pwbd[0:64, l, cp, 0:64] = pw[l, 2 * cp]
            pwbd[64:128, l, cp, 64:128] = pw[l, 2 * cp + 1]
    sh["pwbd"] = pwbd
    sh["pscale"] = f(np.asarray(inp["pool_scale"]).reshape(DEPTH, 2, 128).transpose(2, 0, 1))
    sh["lbraw"] = f(np.asarray(inp["hgrn_lb_raw"]).reshape(DEPTH, 4, 64).transpose(2, 0, 1))
    sh["hnw"] = f(np.asarray(inp["hgrn_norm_w"]).reshape(DEPTH, 4, 64).transpose(2, 0, 1))
    sh["qnw"] = f(np.tile(np.asarray(inp["q_norm_w"]).T, (2, 1)))
    sh["knw"] = f(np.tile(np.asarray(inp["k_norm_w"]).T, (2, 1)))
    sh["sinks"] = f(np.broadcast_to(np.asarray(inp["attn_sinks"])[None], (128, DEPTH, 8)))
    wo = np.asarray(inp["w_out"], dtype=np.float32)
    sh["wo_p"] = f(wo[:, 0:256].reshape(DEPTH, 2, 128, D).transpose(0, 2, 1, 3))
    sh["wo_h"] = f(wo[:, 256:512].reshape(DEPTH, 4, 64, D).transpose(0, 2, 1, 3))
    sh["wo_a"] = f(wo[:, 512:1024].reshape(DEPTH, 4, 128, D).transpose(0, 2, 1, 3))
    sh["rw"] = f(np.asarray(inp["router_w"]).reshape(8, 128, NE).transpose(1, 0, 2))
    sh["rb"] = f(np.broadcast_to(np.asarray(inp["router_bias"])[None], (128, NE)))
    sh["wg"] = f(np.asarray(inp["expert_w_gate"]).reshape(DEPTH, NE, 8, 128, 512).transpose(0, 1, 3, 2, 4))
    sh["wu"] = f(np.asarray(inp["expert_w_up"]).reshape(DEPTH, NE, 8, 128, 512).transpose(0, 1, 3, 2, 4))
    sh["wd"] = f(np.asarray(inp["expert_w_down"]).reshape(DEPTH, NE, 4, 128, D).transpose(0, 1, 3, 2, 4))
    sh.update(_consts())
    return sh


def prep_core(inp, b):
    x = np.asarray(inp["x"][b], dtype=np.float32)
    c = np.asarray(inp["c"][b], dtype=np.float32)
    return {"xT": np.ascontiguousarray(x.T), "c_in": np.ascontiguousarray(c.reshape(8, 128).T)}


_CACHE = {}


def kernel(**inputs):
    if "nc" not in _CACHE:
        _CACHE["nc"] = build_program()[0]
    nc = _CACHE["nc"]
    sh = prep_shared(inputs)
    B = np.asarray(inputs["x"]).shape[0]
    in_maps = []
    for b in range(B):
        m = dict(sh)
        m.update(prep_core(inputs, b))
        in_maps.append(m)
    res = run_bass_kernel_spmd(nc, in_maps, core_ids=list(range(B)))
    out = np.stack([np.ascontiguousarray(res.results[b]["y"].T) for b in range(B)], axis=0)
    return out.astype(np.float32)
```
